# Optimizing a Trainium2 kernel written in Bass

```python
import math
import jax
import jax.numpy as jnp
from jax import lax
import numpy as np

D_MODEL = 2048
BATCH = 8
SEQ = 2048
DEPTH = 4

D_MIX = D_MODEL
HEAD_DIM = 64
SSD_WIDTH = D_MIX // 2
RWKV_WIDTH = D_MIX - SSD_WIDTH
SSD_HEADS = SSD_WIDTH // HEAD_DIM
SSD_GROUPS = 2
SSD_HEADS_PER_GROUP = SSD_HEADS // SSD_GROUPS
SSD_STATE = 128
SSD_CONV_WIDTH = 4
SSD_CHUNK = 128
SSD_CONV_DIM = SSD_WIDTH + 2 * SSD_GROUPS * SSD_STATE
RWKV_HEADS = RWKV_WIDTH // HEAD_DIM
W_LORA = 64
A_LORA = 64
V_LORA = 32
G_LORA = 160
RWKV_SHIFT_DIM = 3 * RWKV_WIDTH + W_LORA + A_LORA + G_LORA
IN_DIM = SSD_WIDTH + SSD_CONV_DIM + SSD_HEADS + RWKV_SHIFT_DIM
D_FF = 11 * D_MODEL // 4
N_EXPERTS = 8
TOP_K = 2
D_FF_EXPERT = D_FF // 2
N_DENSE = (DEPTH + 1) // 2
N_MOE = DEPTH // 2
DEEPNORM_ALPHA = (2 * DEPTH) ** 0.25
DEEPNORM_BETA = (8 * DEPTH) ** -0.25
LN_EPS = 1e-5
RMS_EPS = 1e-5
RWKV_GN_EPS = 64e-5
L2_EPS = 1e-12

kernel_name = "hymba_ssd_rwkv7_deepnorm_moe"


def _split(u, sizes):
    idx = []
    acc = 0
    for s in sizes:
        acc += s
        idx.append(acc)
    return jnp.split(u, idx, axis=-1)


def layer_norm(x, g, b):
    xf = x.astype(jnp.float32)
    mu = jnp.mean(xf, axis=-1, keepdims=True)
    var = jnp.mean(jnp.square(xf - mu), axis=-1, keepdims=True)
    y = (xf - mu) * lax.rsqrt(var + LN_EPS)
    return (y * g + b).astype(x.dtype)


def token_shift(u):
    return jnp.pad(u[:, :-1], ((0, 0), (1, 0), (0, 0)))


def causal_depthwise_conv(u, w, b):
    k = w.shape[0]
    out = lax.conv_general_dilated(
        u, w[:, None, :].astype(u.dtype), window_strides=(1,), padding=[(k - 1, 0)],
        dimension_numbers=("NWC", "WIO", "NWC"), feature_group_count=u.shape[-1])
    return out + b


def ssd_chunked_scan(xdt, dA, bm, cm):
    bsz, t, g, e, p = xdt.shape
    n = bm.shape[-1]
    L = SSD_CHUNK
    nc = t // L
    xc = xdt.reshape(bsz, nc, L, g, e, p)
    bc = bm.reshape(bsz, nc, L, g, n)
    cc = cm.reshape(bsz, nc, L, g, n)
    a = jnp.transpose(dA.reshape(bsz, nc, L, g, e), (0, 1, 3, 4, 2))
    a_cum = jnp.cumsum(a, axis=-1)
    causal = jnp.tril(jnp.ones((L, L), dtype=bool))
    seg = a_cum[..., :, None] - a_cum[..., None, :]
    decay_ls = jnp.exp(jnp.where(causal, seg, -jnp.inf))
    cb = jnp.einsum("bclgn,bcsgn->bcgls", cc, bc)
    y_diag = jnp.einsum("bcgels,bcsgep->bclgep", cb[:, :, :, None] * decay_ls, xc)
    decay_to_end = jnp.exp(a_cum[..., -1:] - a_cum)
    chunk_states = jnp.einsum("bcsgn,bcges,bcsgep->bcgepn", bc, decay_to_end, xc)
    chunk_decay = jnp.exp(a_cum[..., -1])

    def step(h, inp):
        dec, st = inp
        return h * dec[..., None, None] + st, h

    h0 = jnp.zeros((bsz, g, e, p, n), xdt.dtype)
    _, h_prev = lax.scan(step, h0, (jnp.moveaxis(chunk_decay, 1, 0), jnp.moveaxis(chunk_states, 1, 0)))
    h_prev = jnp.moveaxis(h_prev, 0, 1)
    y_off = jnp.einsum("bclgn,bcgepn,bcgel->bclgep", cc, h_prev, jnp.exp(a_cum))
    return (y_diag + y_off).reshape(bsz, t, g, e, p)


def ssd_mixer(z, xbc, dt_raw, conv_w, conv_b, dt_bias, a_log, d_skip, norm_w):
    f32 = jnp.float32
    bsz, t, _ = z.shape
    xbc = jax.nn.silu(causal_depthwise_conv(xbc, conv_w, conv_b))
    xs, bm, cm = _split(xbc, (SSD_WIDTH, SSD_GROUPS * SSD_STATE))
    xs = xs.astype(f32).reshape(bsz, t, SSD_GROUPS, SSD_HEADS_PER_GROUP, HEAD_DIM)
    bm = bm.astype(f32).reshape(bsz, t, SSD_GROUPS, SSD_STATE)
    cm = cm.astype(f32).reshape(bsz, t, SSD_GROUPS, SSD_STATE)
    dt = jax.nn.softplus(dt_raw.astype(f32) + dt_bias.astype(f32)).reshape(bsz, t, SSD_GROUPS, SSD_HEADS_PER_GROUP)
    a = -jnp.exp(a_log.astype(f32)).reshape(SSD_GROUPS, SSD_HEADS_PER_GROUP)
    y = ssd_chunked_scan(xs * dt[..., None], dt * a, bm, cm)
    y = y + xs * d_skip.astype(f32).reshape(SSD_GROUPS, SSD_HEADS_PER_GROUP, 1)
    u = y.reshape(bsz, t, SSD_WIDTH) * jax.nn.silu(z.astype(f32))
    u = u.reshape(bsz, t, SSD_GROUPS, SSD_WIDTH // SSD_GROUPS)
    u = u * lax.rsqrt(jnp.mean(jnp.square(u), axis=-1, keepdims=True) + RMS_EPS)
    return (u.reshape(bsz, t, SSD_WIDTH) * norm_w).astype(z.dtype)


def wkv7_scan(r, w, k, v, a, b):
    bsz, t, h, n = r.shape

    def step(s, inp):
        r_t, w_t, k_t, v_t, a_t, b_t = inp
        sa = jnp.einsum("bhij,bhj->bhi", s, a_t)
        s = s * w_t[:, :, None, :] + sa[..., None] * b_t[:, :, None, :] + v_t[..., None] * k_t[:, :, None, :]
        return s, jnp.einsum("bhij,bhj->bhi", s, r_t)

    s0 = jnp.zeros((bsz, h, n, n), jnp.float32)
    seq = tuple(jnp.moveaxis(u, 1, 0) for u in (r, w, k, v, a, b))
    _, y = lax.scan(step, s0, seq)
    return jnp.moveaxis(y, 0, 1)


def rwkv7_mixer(p, mix, w0, w_up, a0, a_up, g_up, k_k, k_a, r_k, ln_w, ln_b, v_res):
    f32 = jnp.float32
    bsz, t, _ = p.shape
    p = p + (token_shift(p) - p) * mix
    r, k, v, w_lo, a_lo, g_lo = _split(p, (RWKV_WIDTH, RWKV_WIDTH, RWKV_WIDTH, W_LORA, A_LORA))
    w = -jax.nn.softplus(-(w0 + jnp.tanh(w_lo) @ w_up).astype(f32)) - 0.5
    decay = jnp.exp(-jnp.exp(w))
    if v_res is None:
        v_first = v
    else:
        v_lo, v_mix, v0, v_up, v_first = v_res
        v_lo = v_lo + (token_shift(v_lo) - v_lo) * v_mix
        v = v + (v_first - v) * jax.nn.sigmoid(v0 + v_lo @ v_up)
    a = jax.nn.sigmoid(a0 + a_lo @ a_up)
    g = jax.nn.sigmoid(g_lo) @ g_up

    def heads(u):
        return u.astype(f32).reshape(bsz, t, RWKV_HEADS, HEAD_DIM)

    kk = heads(k * k_k)
    kk = kk / jnp.maximum(jnp.linalg.norm(kk, axis=-1, keepdims=True), L2_EPS)
    k = k * (1 + (a - 1) * k_a)
    rh, kh, vh, ah = heads(r), heads(k), heads(v), heads(a)
    y = wkv7_scan(rh, heads(decay), kh, vh, -kk, kk * ah)
    mu = jnp.mean(y, axis=-1, keepdims=True)
    var = jnp.mean(jnp.square(y - mu), axis=-1, keepdims=True)
    y = ((y - mu) * lax.rsqrt(var + RWKV_GN_EPS)).reshape(bsz, t, RWKV_WIDTH) * ln_w + ln_b
    bonus = jnp.sum(rh * kh * r_k, axis=-1, keepdims=True) * vh
    y = y + bonus.reshape(bsz, t, RWKV_WIDTH)
    return (y * g).astype(p.dtype), v_first


def swiglu(h, w1, w3, w2):
    return (jax.nn.silu(h @ w1) * (h @ w3)) @ w2


def moe_swiglu(h, router, w1, w3, w2):
    bsz, t, d = h.shape
    tok = h.reshape(bsz * t, d)
    logits = (tok @ router).astype(jnp.float32)
    top_logits, top_idx = lax.top_k(logits, TOP_K)
    gates = jax.nn.softmax(top_logits, axis=-1)
    combine = jnp.sum(jax.nn.one_hot(top_idx, N_EXPERTS, dtype=jnp.float32) * gates[..., None], axis=1).astype(h.dtype)
    out = jnp.zeros_like(tok)
    for e in range(N_EXPERTS):
        out = out + combine[:, e:e + 1] * swiglu(tok, w1[e], w3[e], w2[e])
    return out.reshape(bsz, t, d)


def setup_inputs(seed: int = 0) -> dict:
    key = jax.random.key(seed)
    ks = iter(jax.random.split(key, 64))
    f32 = jnp.float32

    def nrm(shape, scale):
        return scale * jax.random.normal(next(ks), shape, f32)

    def gain(shape):
        return 1.0 + nrm(shape, 0.02)

    x = nrm((BATCH, SEQ, D_MODEL), 1.0)
    w_in = nrm((DEPTH, D_MODEL, IN_DIM), D_MODEL ** -0.5)
    w_in_vres = nrm((DEPTH - 1, D_MODEL, V_LORA), D_MODEL ** -0.5)
    ssd_conv_w = nrm((DEPTH, SSD_CONV_WIDTH, SSD_CONV_DIM), SSD_CONV_WIDTH ** -0.5)
    ssd_conv_b = nrm((DEPTH, SSD_CONV_DIM), 0.02)
    dt0 = jnp.exp(jax.random.uniform(next(ks), (DEPTH, SSD_HEADS), f32, minval=math.log(1e-3), maxval=math.log(1e-1)))
    ssd_dt_bias = dt0 + jnp.log(-jnp.expm1(-dt0))
    ssd_a_log = jnp.log(jax.random.uniform(next(ks), (DEPTH, SSD_HEADS), f32, minval=1.0, maxval=16.0))
    ssd_d = gain((DEPTH, SSD_HEADS))
    ssd_norm_w = gain((DEPTH, SSD_WIDTH))
    rw_mix = jax.random.uniform(next(ks), (DEPTH, RWKV_SHIFT_DIM), f32)
    rw_vres_mix = jax.random.uniform(next(ks), (DEPTH - 1, V_LORA), f32)
    ratio = jnp.linspace(0.0, 1.0, RWKV_WIDTH, dtype=f32)
    rw_w0 = (-6.0 + 5.0 * ratio ** 0.85 + 0.5)[None, :] + nrm((DEPTH, RWKV_WIDTH), 0.05)
    rw_w_up = nrm((DEPTH, W_LORA, RWKV_WIDTH), 0.5 * W_LORA ** -0.5)
    rw_a0 = nrm((DEPTH, RWKV_WIDTH), 0.1)
    rw_a_up = nrm((DEPTH, A_LORA, RWKV_WIDTH), A_LORA ** -0.5)
    rw_v0 = nrm((DEPTH - 1, RWKV_WIDTH), 0.1)
    rw_v_up = nrm((DEPTH - 1, V_LORA, RWKV_WIDTH), V_LORA ** -0.5)
    rw_g_up = nrm((DEPTH, G_LORA, RWKV_WIDTH), G_LORA ** -0.5)
    rw_k_k = 0.85 + nrm((DEPTH, RWKV_WIDTH), 0.02)
    rw_k_a = gain((DEPTH, RWKV_WIDTH))
    rw_r_k = nrm((DEPTH, RWKV_HEADS, HEAD_DIM), 0.1)
    rw_ln_w = gain((DEPTH, RWKV_WIDTH))
    rw_ln_b = nrm((DEPTH, RWKV_WIDTH), 0.02)
    w_out = nrm((DEPTH, D_MIX, D_MODEL), DEEPNORM_BETA * D_MIX ** -0.5)
    ln1_g = gain((DEPTH, D_MODEL))
    ln1_b = nrm((DEPTH, D_MODEL), 0.02)
    ln2_g = gain((DEPTH, D_MODEL))
    ln2_b = nrm((DEPTH, D_MODEL), 0.02)
    ffn_w1 = nrm((N_DENSE, D_MODEL, D_FF), D_MODEL ** -0.5)
    ffn_w3 = nrm((N_DENSE, D_MODEL, D_FF), D_MODEL ** -0.5)
    ffn_w2 = nrm((N_DENSE, D_FF, D_MODEL), DEEPNORM_BETA * D_FF ** -0.5)
    moe_router = nrm((N_MOE, D_MODEL, N_EXPERTS), D_MODEL ** -0.5)
    moe_w1 = nrm((N_MOE, N_EXPERTS, D_MODEL, D_FF_EXPERT), D_MODEL ** -0.5)
    moe_w3 = nrm((N_MOE, N_EXPERTS, D_MODEL, D_FF_EXPERT), D_MODEL ** -0.5)
    moe_w2 = nrm((N_MOE, N_EXPERTS, D_FF_EXPERT, D_MODEL), DEEPNORM_BETA * D_FF_EXPERT ** -0.5)
    return {
        "x": x, "w_in": w_in, "w_in_vres": w_in_vres,
        "ssd_conv_w": ssd_conv_w, "ssd_conv_b": ssd_conv_b, "ssd_dt_bias": ssd_dt_bias,
        "ssd_a_log": ssd_a_log, "ssd_d": ssd_d, "ssd_norm_w": ssd_norm_w,
        "rw_mix": rw_mix, "rw_vres_mix": rw_vres_mix, "rw_w0": rw_w0, "rw_w_up": rw_w_up,
        "rw_a0": rw_a0, "rw_a_up": rw_a_up, "rw_v0": rw_v0, "rw_v_up": rw_v_up,
        "rw_g_up": rw_g_up, "rw_k_k": rw_k_k, "rw_k_a": rw_k_a, "rw_r_k": rw_r_k,
        "rw_ln_w": rw_ln_w, "rw_ln_b": rw_ln_b, "w_out": w_out,
        "ln1_g": ln1_g, "ln1_b": ln1_b, "ln2_g": ln2_g, "ln2_b": ln2_b,
        "ffn_w1": ffn_w1, "ffn_w3": ffn_w3, "ffn_w2": ffn_w2,
        "moe_router": moe_router, "moe_w1": moe_w1, "moe_w3": moe_w3, "moe_w2": moe_w2,
    }


def reference(x, w_in, w_in_vres, ssd_conv_w, ssd_conv_b, ssd_dt_bias, ssd_a_log, ssd_d, ssd_norm_w,
              rw_mix, rw_vres_mix, rw_w0, rw_w_up, rw_a0, rw_a_up, rw_v0, rw_v_up, rw_g_up,
              rw_k_k, rw_k_a, rw_r_k, rw_ln_w, rw_ln_b, w_out, ln1_g, ln1_b, ln2_g, ln2_b,
              ffn_w1, ffn_w3, ffn_w2, moe_router, moe_w1, moe_w3, moe_w2):
    v_first = None
    for l in range(DEPTH):
        if l == 0:
            w_comb = w_in[0]
        else:
            w_comb = jnp.concatenate([w_in[l], w_in_vres[l - 1]], axis=-1)
        proj = jnp.einsum("btd,dc->btc", x, w_comb)
        z, xbc, dt_raw, rw_p, v_lo = _split(proj, (SSD_WIDTH, SSD_CONV_DIM, SSD_HEADS, RWKV_SHIFT_DIM))
        y_ssd = ssd_mixer(z, xbc, dt_raw, ssd_conv_w[l], ssd_conv_b[l], ssd_dt_bias[l], ssd_a_log[l],
                          ssd_d[l], ssd_norm_w[l])
        if l == 0:
            v_res = None
        else:
            v_res = (v_lo, rw_vres_mix[l - 1], rw_v0[l - 1], rw_v_up[l - 1], v_first)
        y_rw, v_first = rwkv7_mixer(rw_p, rw_mix[l], rw_w0[l], rw_w_up[l], rw_a0[l], rw_a_up[l], rw_g_up[l],
                                    rw_k_k[l], rw_k_a[l], rw_r_k[l], rw_ln_w[l], rw_ln_b[l], v_res)
        mixed = jnp.einsum("btc,cd->btd", jnp.concatenate([y_ssd, y_rw], axis=-1), w_out[l])
        x = layer_norm(DEEPNORM_ALPHA * x + mixed, ln1_g[l], ln1_b[l])
        if l % 2 == 0:
            f = swiglu(x, ffn_w1[l // 2], ffn_w3[l // 2], ffn_w2[l // 2])
        else:
            f = moe_swiglu(x, moe_router[l // 2], moe_w1[l // 2], moe_w3[l // 2], moe_w2[l // 2])
        x = layer_norm(DEEPNORM_ALPHA * x + f, ln2_g[l], ln2_b[l])
    return x
```

```python
import contextlib
import numpy as np
import concourse.bass as bass
import concourse.mybir as mybir
from concourse.bass_utils import run_bass_kernel_spmd

F32 = mybir.dt.float32
BF16 = mybir.dt.bfloat16
AF = mybir.ActivationFunctionType
ALU = mybir.AluOpType
AX = mybir.AxisListType

TT = 2048
D = 2048
NL = 4
IN_DIM = 5936
DFF = 5632
DFE = 2816
ALPHA = float((2 * NL) ** 0.25)
NPP = 137
NBV = 11280
CW = 4738
BV_D, BV_NW, BV_LW, BV_LB, BV_G1, BV_B1, BV_G2, BV_B2 = 0, 16, 1040, 2064, 3088, 5136, 7184, 9232
C_ID, C_MI, C_MS, C_ML, C_BO, C_S2, C_S16, C_RM = 0, 128, 256, 384, 512, 640, 642, 2690


class Buf:
    __slots__ = ("name", "w", "r")

    def __init__(self, name):
        self.name = name
        self.w = []
        self.r = []


class V:
    __slots__ = ("ap", "bufs")

    def __init__(self, ap, bufs):
        self.ap = ap
        self.bufs = bufs


class T:
    def __init__(self, handle, name):
        self.h = handle
        self.buf = Buf(name)
        self.sub = {}

    def __getitem__(self, idx):
        return V(self.h[idx], [self.buf])

    def v(self, ap):
        return V(ap, [self.buf])

    def kb(self, key):
        b = self.sub.get(key)
        if b is None:
            b = self.sub[key] = Buf(f"{self.buf.name}.{key}")
        return b

    def k(self, keys, ap):
        if not isinstance(keys, (list, tuple)):
            keys = [keys]
        return V(ap, [self.kb(k) for k in keys])


class DT:
    def __init__(self, nc, name, shape, dtype, kind="Internal"):
        self.h = nc.dram_tensor(name, list(shape), dtype, kind=kind)
        self.ap = self.h.ap()
        self.name = name
        self.bufs = {}

    def v(self, ap, *keys):
        bs = []
        for key in (keys or (None,)):
            b = self.bufs.get(key)
            if b is None:
                b = self.bufs[key] = Buf(f"{self.name}.{key}")
            bs.append(b)
        return V(ap, bs)


class Prog:
    NDMA = 8

    def __init__(self, nc, stack):
        self.nc = nc
        self.stack = stack
        self.eng = {"pe": nc.tensor, "act": nc.scalar, "dve": nc.vector, "pool": nc.gpsimd, "sp": nc.sync}
        self.csem = {}
        self.ccount = {}
        for e in ("pe", "act", "dve", "pool"):
            self.csem[e] = stack.enter_context(nc.semaphore(f"c_{e}"))
            self.ccount[e] = 0
        self.dsem = {}
        self.dcount = {}
        for q in ("sp", "pool"):
            self.dsem[q] = [stack.enter_context(nc.semaphore(f"d_{q}{i}")) for i in range(self.NDMA)]
            self.dcount[q] = 0
        self.waited = {e: {} for e in self.eng}
        self.nops = 0

    def sb(self, st, name, shape, dtype):
        self.nalloc = getattr(self, "nalloc", 0) + 1
        name = f"{name}_{self.nalloc}"
        return T(st.enter_context(self.nc.sbuf_tensor(name, list(shape), dtype)), name)

    def ps(self, st, name, shape, dtype=F32):
        self.nalloc = getattr(self, "nalloc", 0) + 1
        name = f"{name}_{self.nalloc}"
        return T(st.enter_context(self.nc.psum_tensor(name, list(shape), dtype)), name)

    def _sem(self, key):
        kind, q, i = key
        return self.csem[q] if kind == "c" else self.dsem[q][i]

    def _wait(self, e, tokens):
        eng = self.eng[e]
        need = {}
        for (key, val) in tokens:
            if key[0] == "c" and key[1] == e and e == "pe":
                continue
            if self.waited[e].get(key, 0) >= val:
                continue
            if need.get(key, 0) < val:
                need[key] = val
        for key, val in need.items():
            eng.wait_ge(self._sem(key), val)
            self.waited[e][key] = val

    @staticmethod
    def _deps(reads, writes):
        toks = []
        for v in reads:
            for b in v.bufs:
                toks += b.w
        for v in writes:
            for b in v.bufs:
                toks += b.w
                toks += b.r
        return toks

    @staticmethod
    def _mark(reads, writes, tok):
        for v in reads:
            for b in v.bufs:
                for i_, (k_, v_) in enumerate(b.r):
                    if k_ == tok[0]:
                        if v_ < tok[1]:
                            b.r[i_] = tok
                        break
                else:
                    b.r.append(tok)
        for v in writes:
            for b in v.bufs:
                b.w = [tok]
                b.r = []

    def op(self, e, fn, reads=(), writes=(), sig=True):
        self._wait(e, self._deps(reads, writes))
        ins = fn(self.eng[e])
        self.nops += 1
        if sig:
            self.ccount[e] += 1
            ins.then_inc(self.csem[e], 1)
            tok = (("c", e, 0), self.ccount[e])
        else:
            tok = (("c", e, 0), self.ccount[e] + 1)
        self._mark(reads, writes, tok)
        return ins

    def dma(self, q, out, in_, **kw):
        k = self.dcount[q]
        self.dcount[q] += 1
        i = k % self.NDMA
        gen = k // self.NDMA
        key = ("d", q, i)
        toks = [(key, 16 * gen)] if gen > 0 else []
        toks += self._deps([in_], [out])
        self._wait(q, toks)
        ins = self.eng[q].dma_start(out=out.ap, in_=in_.ap, **kw)
        ins.then_inc(self.dsem[q][i], 16)
        self.nops += 1
        self._mark([in_], [out], (key, 16 * (gen + 1)))
        return ins

    def barrier(self):
        toks = [(("c", e, 0), self.ccount[e]) for e in self.csem if self.ccount[e] > 0]
        for q in self.dsem:
            k = self.dcount[q]
            for i in range(self.NDMA):
                n = (k - i + self.NDMA - 1) // self.NDMA if k > i else 0
                if n > 0:
                    toks.append((("d", q, i), 16 * n))
        for e in self.eng:
            self._wait(e, toks)

    def mm(self, out, lhsT, rhs, start=True, stop=True, sig=None):
        if sig is None:
            sig = stop
        return self.op("pe", lambda t: t.matmul(out.ap, lhsT.ap, rhs.ap, start=start, stop=stop),
                       reads=[lhsT, rhs], writes=[out], sig=sig)

    def tr(self, out, in_, ident, sig=True):
        return self.op("pe", lambda t: t.transpose(out.ap, in_.ap, ident.ap), reads=[in_, ident], writes=[out], sig=sig)

    def activation(self, out, in_, func, bias=None, scale=None, accum_out=None):
        reads = [in_]
        kw = {}
        if bias is not None:
            if isinstance(bias, V):
                reads.append(bias); kw["bias"] = bias.ap
            else:
                kw["bias"] = bias
        if scale is not None:
            if isinstance(scale, V):
                reads.append(scale); kw["scale"] = scale.ap
            else:
                kw["scale"] = scale
        writes = [out]
        if accum_out is not None:
            writes.append(accum_out); kw["accum_out"] = accum_out.ap
        return self.op("act", lambda s: s.activation(out.ap, in_.ap, func, **kw), reads=reads, writes=writes)

    def tt(self, out, in0, in1, op, e="dve"):
        return self.op(e, lambda v: v.tensor_tensor(out.ap, in0.ap, in1.ap, op), reads=[in0, in1], writes=[out])

    def ts(self, out, in0, s1, op0, s2=None, op1=None, e="dve"):
        reads = [in0]
        a1 = s1
        if isinstance(s1, V):
            reads.append(s1); a1 = s1.ap
        a2 = s2
        if isinstance(s2, V):
            reads.append(s2); a2 = s2.ap
        kw = {}
        if op1 is not None:
            kw["op1"] = op1
        return self.op(e, lambda v: v.tensor_scalar(out.ap, in0.ap, a1, a2, op0, **kw), reads=reads, writes=[out])

    def stt(self, out, in0, scalar, in1, op0, op1, accum_out=None):
        reads = [in0, in1]
        a = scalar
        if isinstance(scalar, V):
            reads.append(scalar); a = scalar.ap
        writes = [out]
        kw = {}
        if accum_out is not None:
            writes.append(accum_out); kw["accum_out"] = accum_out.ap
        return self.op("dve", lambda v: v.scalar_tensor_tensor(out.ap, in0.ap, a, in1.ap, op0, op1, **kw),
                       reads=reads, writes=writes)

    def copy(self, out, in_, e="dve"):
        if e == "act":
            return self.op(e, lambda s: s.copy(out.ap, in_.ap), reads=[in_], writes=[out])
        return self.op(e, lambda v: v.tensor_copy(out.ap, in_.ap), reads=[in_], writes=[out])

    def memset(self, out, val, e="dve"):
        return self.op(e, lambda v: v.memset(out.ap, val), reads=[], writes=[out])


def r3(ap, q):
    return ap.rearrange("p (e q) -> p e q", q=q)


def bc3(ap, n):
    return ap.unsqueeze(2).to_broadcast([ap.shape[0], ap.shape[1], n])


class Ctx:
    pass


def build_program(n_layers=NL, dbg=(), phases="isrof"):
    nc = bass.Bass("TRN2", target_bir_lowering=False)
    g = Ctx()
    g.nc = nc

    def din(name, shape):
        return DT(nc, name, shape, F32, kind="ExternalInput")

    def scratch(name, shape, dtype):
        return DT(nc, name, shape, dtype, kind=("ExternalOutput" if name in dbg else "Internal"))

    g.x = din("x", [TT, D])
    g.w_in = din("w_in", [NL, D, IN_DIM])
    g.w_vres = din("w_in_vres", [NL - 1, D, 32])
    g.w_up = din("rw_w_up", [NL, 64, 1024])
    g.a_up = din("rw_a_up", [NL, 64, 1024])
    g.v_up = din("rw_v_up", [NL - 1, 32, 1024])
    g.g_up = din("rw_g_up", [NL, 160, 1024])
    g.w_out = din("w_out", [NL, D, D])
    g.ffn_w1 = din("ffn_w1", [2, D, DFF])
    g.ffn_w3 = din("ffn_w3", [2, D, DFF])
    g.ffn_w2 = din("ffn_w2", [2, DFF, D])
    g.moe_w1 = din("moe_w1", [2, 8, D, DFE])
    g.moe_w3 = din("moe_w3", [2, 8, D, DFE])
    g.moe_w2 = din("moe_w2", [2, 8, DFE, D])
    g.routerT = din("routerT", [2, 8, D])
    g.pp = din("pp", [NL, 128, NPP])
    g.bv = din("bv", [NL, NBV])
    g.consts = din("consts", [128, CW])
    g.out = DT(nc, "out", [TT, D], F32, kind="ExternalOutput")

    g.xres = scratch("xres", [TT, D], F32)
    g.xT_d = scratch("xT_d", [D, TT], BF16)
    g.zs_d = scratch("zs_d", [TT, 1024], F32)
    g.xs_d = scratch("xs_d", [TT, 1024], F32)
    g.BT_d = scratch("BT_d", [256, TT], BF16)
    g.CT_d = scratch("CT_d", [256, TT], BF16)
    g.Btok_d = scratch("Btok_d", [TT, 256], BF16)
    g.dtT_d = scratch("dtT_d", [16, TT], F32)
    g.acT_d = scratch("acT_d", [16, TT], F32)
    g.dttok_d = scratch("dttok_d", [TT, 32], F32)
    g.pT_d = scratch("pT_d", [3456, TT], F32)
    g.vfT_d = scratch("vfT_d", [1024, TT], F32)
    g.yT_d = scratch("yT_d", [D, TT], BF16)
    g.comb_d = scratch("comb_d", [TT, 8], F32)

    with contextlib.ExitStack() as st0:
        P = Prog(nc, st0)
        g.P = P
        g.cst = P.sb(st0, "cst", [128, C_S16], F32)
        g.identb = P.sb(st0, "identb", [128, 128], BF16)
        P.dma("sp", g.cst[:], g.consts.v(g.consts.ap[:, 0:C_S16]))
        P.copy(g.identb[:], g.cst[:, C_ID:C_ID + 128])
        g.ident = g.cst[:, C_ID:C_ID + 128]

        phase_ln_transpose_init(g)
        for l in range(n_layers):
            if "i" in phases:
                phase_inproj(g, l)
            if "s" in phases:
                phase_ssd(g, l)
            if "r" in phases:
                phase_rwkv(g, l)
            if "o" in phases:
                phase_wout_ln(g, l)
            if "f" in phases:
                phase_ffn(g, l, last=(l == n_layers - 1))
        P.barrier()
    return nc


def emit_transposes_bf16(g, P, src_bf, pst, dst_view_fn, nblk):
    for c in range(nblk):
        P.tr(pst[:, c * 128:(c + 1) * 128], src_bf[:, c * 128:(c + 1) * 128], g.identb[:], sig=(c == nblk - 1))


def phase_ln_transpose_init(g):
    P = g.P
    with contextlib.ExitStack() as st:
        xin = [P.sb(st, f"i_x{i}", [128, D], F32) for i in range(2)]
        xb = [P.sb(st, f"i_xb{i}", [128, D], BF16) for i in range(2)]
        xo = [P.sb(st, f"i_xo{i}", [128, 16, 128], BF16) for i in range(2)]
        pst = [P.ps(st, f"i_ps{i}", [128, D], BF16) for i in range(2)]
        for tt in range(16):
            i = tt % 2
            P.dma("sp", xin[i][:], g.x.v(g.x.ap[tt * 128:(tt + 1) * 128, :], tt))
            P.copy(xb[i][:], xin[i][:], e="act")
            emit_transposes_bf16(g, P, xb[i], pst[i], None, 16)
            P.copy(xo[i].v(xo[i].h[:].rearrange("p c t -> p (c t)")), pst[i][:])
            P.dma("sp", g.xT_d.v(g.xT_d.ap.rearrange("(c p) t -> p c t", p=128)[:, :, tt * 128:(tt + 1) * 128], tt), xo[i][:])
        P.barrier()


def load_wblock(P, wt, src_ap, ncols):
    s = src_ap.rearrange("(c p) m -> p c m", p=128)
    for hf in range(2):
        P.dma("pool", wt.v(wt.h[:, hf * 8:(hf + 1) * 8, 0:ncols]), V(s[:, hf * 8:(hf + 1) * 8, :], []))


def phase_inproj(g, l):
    P = g.P
    with contextlib.ExitStack() as st:
        xT = P.sb(st, "p_xT", [128, 16, TT], BF16)
        wt = [P.sb(st, f"p_w{i}", [128, 16, 512], BF16) for i in range(2)]
        pp = P.sb(st, "p_pp", [128, NPP], F32)
        bigh = [P.ps(st, f"p_big{i}", [128, 1024], F32) for i in range(2)]
        ptr = P.ps(st, "p_trf", [128, 1024], F32)
        pbt = P.ps(st, "p_trb", [128, 2048], BF16)
        ub = [P.sb(st, f"p_u{i}", [128, 4 + TT], F32) for i in range(2)]
        acc = P.sb(st, "p_acc", [128, TT], F32)
        ob = [P.sb(st, f"p_ob{i}", [128, TT], F32) for i in range(2)]
        obb = P.sb(st, "p_obb", [128, TT], BF16)
        tk = P.sb(st, "p_tk", [128, 16, 128], F32)
        tkb = P.sb(st, "p_tkb", [128, 16, 128], BF16)
        rmask = P.sb(st, "p_rmask", [16, TT], F32)
        negA = P.sb(st, "p_negA", [16, 1], F32)
        dtt = P.sb(st, "p_dtt", [16, TT], F32)
        dat = P.sb(st, "p_dat", [16, TT], F32)
        act_ = P.sb(st, "p_act", [16, TT], F32)
        dtk = P.sb(st, "p_dtk", [128, 16, 32], F32)

        for hf in range(4):
            P.dma("sp", xT.v(xT.h[:, hf * 4:(hf + 1) * 4, :]),
                  g.xT_d.v(g.xT_d.ap.rearrange("(c p) t -> p c t", p=128)[:, hf * 4:(hf + 1) * 4, :], *range(16)))
        P.dma("sp", pp[:], g.pp.v(g.pp.ap[l]))
        P.dma("sp", rmask[:], g.consts.v(g.consts.ap[0:16, C_RM:C_RM + TT]))
        for i in range(2):
            P.memset(ub[i][:, 0:4], 0.0)

        blocks = [(0, 512), (512, 512), (1024, 512), (1536, 512), (2048, 512), (2560, 16)]
        blocks += [(2576 + 512 * i, 512) for i in range(6)] + [(5648, 288)]
        nblk = len(blocks)
        wi = g.w_in.ap[l]

        def issue_load(bi):
            c0, n = blocks[bi]
            t = wt[bi % 2]
            load_wblock(P, t, wi[:, c0:c0 + n], n)
            if bi == nblk - 1 and l > 0:
                s = g.w_vres.ap[l - 1].rearrange("(c p) m -> p c m", p=128)
                P.dma("pool", t.v(t.h[:, :, n:n + 32]), V(s, []))

        chunks = []
        for j in range(8):
            chunks.append((j * 128, 128, "z", j))
        for j in range(12):
            chunks.append((1024 + j * 128, 128, "conv", j))
        chunks.append((2560, 16, "dt", 0))
        for j in range(26):
            chunks.append((2576 + j * 128, 128, "rw", j))
        chunks.append((2576 + 26 * 128, 32 + (32 if l > 0 else 0), "rw", 26))

        def blk_of(c0):
            for bi, (b0, n) in enumerate(blocks):
                if b0 <= c0 < b0 + n:
                    return bi
            raise AssertionError(c0)

        issue_load(0)
        loaded = 1
        HS = 1024
        deferred = []
        for (c0, wdt, kind, idx) in chunks:
            bi = blk_of(c0)
            off = c0 - blocks[bi][0]
            while loaded <= bi + 1 and loaded < nblk:
                issue_load(loaded)
                loaded += 1
            w_ = wt[bi % 2]
            u = ub[idx % 2]
            o = ob[idx % 2]
            for hf in range(2):
                for tq in range(2):
                    t0 = hf * HS + tq * 512
                    for kc in range(16):
                        P.mm(bigh[hf].v(bigh[hf].h[0:wdt, tq * 512:(tq + 1) * 512]), w_.v(w_.h[:, kc, off:off + wdt]),
                             xT.v(xT.h[:, kc, t0:t0 + 512]), start=(kc == 0), stop=(kc == 15))
                src = bigh[hf].v(bigh[hf].h[0:wdt, :])
                if kind == "z":
                    P.activation(o.v(o.h[:, hf * HS:(hf + 1) * HS]), src, AF.Silu)
                elif kind == "dt":
                    P.activation(dtt.v(dtt.h[:, hf * HS:(hf + 1) * HS]), src, AF.Exp, bias=pp[0:16, 60:61])
                else:
                    P.copy(u.v(u.h[0:wdt, 4 + hf * HS:4 + (hf + 1) * HS]), src, e="act")
            for fn_ in deferred:
                fn_()
            deferred = []

            def to_tok_f32_now(srcT, dst_d, col0):
                for half in range(2):
                    for tt in range(8):
                        t_ = half * 8 + tt
                        P.tr(ptr[:, tt * 128:(tt + 1) * 128], srcT[:, t_ * 128:(t_ + 1) * 128], g.ident, sig=(tt == 7))
                    P.copy(tk.v(tk.h[:, half * 8:(half + 1) * 8, :].rearrange("p c t -> p (c t)")), ptr[:, 0:1024])
                P.dma("sp", dst_d.v(dst_d.ap.rearrange("(c p) m -> p c m", p=128)[:, :, col0:col0 + 128], "all"), tk[:])

            def to_tok_f32(srcT, dst_d, col0):
                deferred.append(lambda: to_tok_f32_now(srcT, dst_d, col0))

            if kind == "z":
                to_tok_f32(o, g.zs_d, idx * 128)
            elif kind == "conv":
                cwb = idx * 4
                P.ts(acc[:], u[:, 1:1 + TT], pp[:, cwb:cwb + 1], ALU.mult, pp[:, 48 + idx:49 + idx], ALU.add)
                for k in range(1, 4):
                    P.stt(acc[:], u[:, 1 + k:1 + k + TT], pp[:, cwb + k:cwb + k + 1], acc[:], ALU.mult, ALU.add)
                if idx < 8:
                    P.activation(o[:], acc[:], AF.Silu)
                    to_tok_f32(o, g.xs_d, idx * 128)
                else:
                    P.activation(obb[:], acc[:], AF.Silu)
                    gi = (idx - 8) % 2
                    if idx < 10:
                        P.dma("sp", g.BT_d.v(g.BT_d.ap[gi * 128:(gi + 1) * 128, :], "all"), obb[:])

                        def b_post(gi=gi):
                            for tt in range(16):
                                P.tr(pbt[:, tt * 128:(tt + 1) * 128], obb[:, tt * 128:(tt + 1) * 128], g.identb[:], sig=(tt == 15))
                            P.copy(tkb.v(tkb.h[:].rearrange("p c t -> p (c t)")), pbt[:])
                            P.dma("sp", g.Btok_d.v(g.Btok_d.ap.rearrange("(c p) m -> p c m", p=128)[:, :, gi * 128:(gi + 1) * 128], "all"), tkb[:])
                        deferred.append(b_post)
                    else:
                        P.dma("sp", g.CT_d.v(g.CT_d.ap[gi * 128:(gi + 1) * 128, :], "all"), obb[:])
            elif kind == "dt":
                P.activation(dtt[:], dtt[:], AF.Ln, bias=1.0)
                P.activation(negA[:], pp[0:16, 61:62], AF.Exp)
                P.ts(negA[:], negA[:], -1.0, ALU.mult)
                P.ts(dat[:], dtt[:], negA[:, 0:1], ALU.mult)
                P.op("dve", lambda v: v.tensor_tensor_scan(act_.h[:], rmask.h[:], dat.h[:], 0.0, ALU.mult, ALU.add),
                     reads=[rmask[:], dat[:]], writes=[act_[:]])
                P.dma("sp", g.acT_d.v(g.acT_d.ap, "all"), act_[:])
                for tt in range(16):
                    P.tr(ptr[:, tt * 32:tt * 32 + 16], dtt[:, tt * 128:(tt + 1) * 128], g.cst[0:16, C_ID:C_ID + 16], sig=False)
                    P.tr(ptr[:, tt * 32 + 16:tt * 32 + 32], act_[:, tt * 128:(tt + 1) * 128], g.cst[0:16, C_ID:C_ID + 16], sig=True)
                P.copy(dtk.v(dtk.h[:].rearrange("p c t -> p (c t)")), ptr[:, 0:512])
                P.dma("sp", g.dttok_d.v(g.dttok_d.ap.rearrange("(c p) m -> p c m", p=128), "all"), dtk[:])
            elif kind == "rw":
                mcol = 62 + idx
                P.tt(acc.v(acc.h[0:wdt, :]), u.v(u.h[0:wdt, 3:3 + TT]), u.v(u.h[0:wdt, 4:4 + TT]), ALU.subtract)
                P.stt(o.v(o.h[0:wdt, :]), acc.v(acc.h[0:wdt, :]), pp[0:wdt, mcol:mcol + 1], u.v(u.h[0:wdt, 4:4 + TT]), ALU.mult, ALU.add)
                P.dma("sp", g.pT_d.v(g.pT_d.ap[idx * 128:idx * 128 + wdt, :], idx), o.v(o.h[0:wdt, :]))
        for fn_ in deferred:
            fn_()
        P.barrier()


def phase_ssd(g, l):
    P = g.P
    with contextlib.ExitStack() as st:
        BT = P.sb(st, "s_BT", [128, 2, TT], BF16)
        CT = P.sb(st, "s_CT", [128, 2, TT], BF16)
        acT = P.sb(st, "s_acT", [16, TT], F32)
        sel16 = P.sb(st, "s_sel", [16, 2048], F32)
        bvt = P.sb(st, "s_bv", [128, 1040], F32)
        H = P.sb(st, "s_H", [128, 1024], F32)
        Hb = P.sb(st, "s_Hb", [128, 1024], BF16)
        yT = P.sb(st, "s_yT", [128, 8, TT], BF16)
        xs = [P.sb(st, f"s_xs{i}", [128, 1024], F32) for i in range(2)]
        zs = [P.sb(st, f"s_zs{i}", [128, 1024], F32) for i in range(2)]
        dk = [P.sb(st, f"s_dk{i}", [128, 32], F32) for i in range(2)]
        Bk = [P.sb(st, f"s_Bk{i}", [128, 256], BF16) for i in range(2)]
        xdt = P.sb(st, "s_xdt", [128, 1024], BF16)
        xdte = P.sb(st, "s_xdte", [128, 1024], BF16)
        cbm = P.sb(st, "s_cbm", [128, 2, 128], F32)
        dif = P.sb(st, "s_dif", [128, 8, 128], F32)
        M = P.sb(st, "s_M", [128, 16, 128], BF16)
        alast = P.sb(st, "s_alast", [128, 16], F32)
        expa = P.sb(st, "s_expa", [128, 16], F32)
        dte = P.sb(st, "s_dte", [128, 16], F32)
        cd = P.sb(st, "s_cd", [128, 16], F32)
        wgt = P.sb(st, "s_wgt", [128, 16], F32)
        y1 = P.sb(st, "s_y1", [128, 1024], F32)
        y = P.sb(st, "s_y", [128, 1024], F32)
        tmp = P.sb(st, "s_tmp", [128, 1024], F32)
        ssq = P.sb(st, "s_ssq", [128, 2], F32)
        rstd = P.sb(st, "s_rstd", [128, 2], F32)
        yb = P.sb(st, "s_yb", [128, 1024], BF16)
        psA = P.ps(st, "s_psA", [128, 1024], F32)
        psB = P.ps(st, "s_psB", [128, 1024], F32)
        psC = P.ps(st, "s_psC", [128, 1024], F32)
        psD = P.ps(st, "s_psD", [128, 512], F32)
        psE = P.ps(st, "s_psE", [128, 1024], BF16)

        for gi in range(2):
            P.dma("sp", BT.v(BT.h[:, gi, :]), g.BT_d.v(g.BT_d.ap[gi * 128:(gi + 1) * 128, :], "all"))
            P.dma("sp", CT.v(CT.h[:, gi, :]), g.CT_d.v(g.CT_d.ap[gi * 128:(gi + 1) * 128, :], "all"))
        P.dma("sp", acT[:], g.acT_d.v(g.acT_d.ap, "all"))
        P.dma("sp", sel16[:], g.consts.v(g.consts.ap[0:16, C_S16:C_S16 + 2048]))
        P.dma("sp", bvt[:], g.bv.v(g.bv.ap[l, 0:1040].partition_broadcast(128)))
        P.memset(H[:], 0.0)
        P.memset(Hb[:], 0.0)
        mI = g.cst.h[:, C_MI:C_MI + 128]

        def loads(c):
            i = c % 2
            rows = slice(c * 128, (c + 1) * 128)
            P.dma("sp", xs[i][:], g.xs_d.v(g.xs_d.ap[rows, :], "all"))
            P.dma("sp", zs[i][:], g.zs_d.v(g.zs_d.ap[rows, :], "all"))
            P.dma("sp", dk[i][:], g.dttok_d.v(g.dttok_d.ap[rows, :], "all"))
            P.dma("sp", Bk[i][:], g.Btok_d.v(g.Btok_d.ap[rows, :], "all"))

        loads(0)
        for c in range(16):
            i = c % 2
            if c + 1 < 16:
                loads(c + 1)
            tsl = slice(c * 128, (c + 1) * 128)
            X, Z, DK, BK = xs[i], zs[i], dk[i], Bk[i]
            P.tt(xdt.v(r3(xdt.h[:], 64)), X.v(r3(X.h[:], 64)), DK.v(bc3(DK.h[:, 0:16], 64)), ALU.mult)
            for gi in range(2):
                P.mm(psD[:, gi * 128:(gi + 1) * 128], BT.v(BT.h[:, gi, tsl]), CT.v(CT.h[:, gi, tsl]))
            P.tt(cbm[:], psD.v(psD.h[:, 0:256].rearrange("p (a b) -> p a b", b=128)),
                 g.cst.v(mI.unsqueeze(1).to_broadcast([128, 2, 128])), ALU.mult)
            for hh in range(2):
                for e8 in range(8):
                    e = hh * 8 + e8
                    P.mm(psA[:, e8 * 128:(e8 + 1) * 128], sel16[:, e * 128:(e + 1) * 128], acT[:, tsl])
                pa3 = psA.h[:].rearrange("p (a b) -> p a b", b=128)
                P.tt(dif[:], psA.v(pa3), DK.v(bc3(DK.h[:, 16 + hh * 8:24 + hh * 8], 128)), ALU.subtract)
                P.copy(alast.v(alast.h[:, hh * 8:(hh + 1) * 8].unsqueeze(2)), psA.v(pa3[:, :, 127:128]))
                P.activation(dif[:], dif[:], AF.Exp)
                P.stt(M.v(M.h[:, hh * 8:(hh + 1) * 8, :]), dif[:], 1.0,
                      cbm.v(cbm.h[:, hh, :].unsqueeze(1).to_broadcast([128, 8, 128])), ALU.min, ALU.mult)
            for e in range(16):
                P.mm(psB[:, e * 64:(e + 1) * 64], M.v(M.h[:, e, :]), xdt[:, e * 64:(e + 1) * 64])
            for gi in range(2):
                P.mm(psC[:, gi * 512:(gi + 1) * 512], CT.v(CT.h[:, gi, tsl]), Hb[:, gi * 512:(gi + 1) * 512])
            P.activation(expa[:], DK[:, 16:32], AF.Exp)
            P.tt(y1.v(r3(y1.h[:], 64)), psC.v(r3(psC.h[:], 64)), expa.v(bc3(expa.h[:], 64)), ALU.mult)
            P.tt(y[:], y1[:], psB[:], ALU.add)
            P.tt(tmp.v(r3(tmp.h[:], 64)), X.v(r3(X.h[:], 64)), bvt.v(bc3(bvt.h[:, 0:16], 64)), ALU.mult)
            P.tt(y[:], y[:], tmp[:], ALU.add)
            P.tt(y[:], y[:], Z[:], ALU.mult)
            for gi in range(2):
                P.activation(tmp[:, gi * 512:(gi + 1) * 512], y[:, gi * 512:(gi + 1) * 512], AF.Square, accum_out=ssq[:, gi:gi + 1])
            P.activation(rstd[:], ssq[:], AF.Sqrt, bias=1e-5, scale=1.0 / 512.0)
            P.op("dve", lambda v: v.reciprocal(rstd.h[:], rstd.h[:]), reads=[rstd[:]], writes=[rstd[:]])
            for gi in range(2):
                P.ts(y1[:, gi * 512:(gi + 1) * 512], y[:, gi * 512:(gi + 1) * 512], rstd[:, gi:gi + 1], ALU.mult)
            P.tt(yb[:], y1[:], bvt[:, 16:1040], ALU.mult)
            for j in range(8):
                P.tr(psE[:, j * 128:(j + 1) * 128], yb[:, j * 128:(j + 1) * 128], g.identb[:], sig=(j == 7))
            P.copy(yT.v(yT.h[:, :, tsl]), psE.v(psE.h[:].rearrange("p (a b) -> p a b", b=128)))
            P.tt(dte[:], alast[:], DK[:, 16:32], ALU.subtract)
            P.activation(dte[:], dte[:], AF.Exp)
            P.activation(cd[:], alast[:], AF.Exp)
            P.tt(wgt[:], DK[:, 0:16], dte[:], ALU.mult)
            P.tt(xdte.v(r3(xdte.h[:], 64)), X.v(r3(X.h[:], 64)), wgt.v(bc3(wgt.h[:], 64)), ALU.mult)
            for gi in range(2):
                P.mm(psC[:, gi * 512:(gi + 1) * 512], BK[:, gi * 128:(gi + 1) * 128], xdte[:, gi * 512:(gi + 1) * 512])
            P.tt(H.v(r3(H.h[:], 64)), H.v(r3(H.h[:], 64)), cd.v(bc3(cd.h[:], 64)), ALU.mult)
            P.tt(H[:], H[:], psC[:], ALU.add)
            P.copy(Hb[:], H[:], e="act")
        P.dma("sp", g.yT_d.v(g.yT_d.ap[0:1024, :].rearrange("(c p) t -> p c t", p=128), "ssd"), yT[:])
        P.barrier()


def phase_rwkv(g, l):
    P = g.P
    U = 4
    with contextlib.ExitStack() as st:
        pp = P.sb(st, "r_pp", [128, NPP], F32)
        omk = P.sb(st, "r_omk", [128, 8], F32)
        bvt = P.sb(st, "r_bv", [128, 2048], F32)
        mk4 = P.sb(st, "r_mk4", [128, 512], F32)
        waT = P.sb(st, "r_waT", [128, TT], BF16)
        gAT = P.sb(st, "r_gAT", [128, TT], BF16)
        gBT = P.sb(st, "r_gBT", [64, TT], BF16)
        wa_up = P.sb(st, "r_waup", [128, 1024], BF16)
        gA_up = P.sb(st, "r_gAup", [128, 1024], BF16)
        gB_up = P.sb(st, "r_gBup", [64, 1024], BF16)
        arT = P.sb(st, "r_arT", [128, 16, 2, 128], BF16)
        bTb = P.sb(st, "r_bTb", [128, TT], BF16)
        kTb = P.sb(st, "r_kTb", [128, TT], BF16)
        Btok = P.sb(st, "r_Btok", [128, 16, 128], BF16)
        Ktok = P.sb(st, "r_Ktok", [128, 16, 128], BF16)
        Vtok = P.sb(st, "r_Vtok", [128, 16, 128], F32)
        Vbf = P.sb(st, "r_Vbf", [128, 16, 128], BF16)
        coef = P.sb(st, "r_coef", [128, 16, 2], F32)
        wc = P.sb(st, "r_wc", [128, 16], F32)
        yrwT = P.sb(st, "r_yrwT", [128, TT], BF16)
        rmask = P.sb(st, "r_rmask", [128, TT], F32)
        P.dma("sp", rmask[:], g.consts.v(g.consts.ap[:, C_RM:C_RM + TT]))
        Q = [P.ps(st, f"r_Q{i}", [128, 1024], F32) for i in range(3)]
        B6 = P.ps(st, "r_B6", [128, 512], F32)
        pTb = P.ps(st, "r_pTb", [128, 1024], BF16)

        def qv(qi, ap_fn, keys):
            return Q[qi].k(keys, ap_fn(Q[qi].h))

        psL = lambda sl=slice(0, 1024): qv(0, lambda h: h[:, sl], [0] if sl.stop <= 512 else ([1] if sl.start >= 512 else [0, 1]))

        P.dma("sp", pp[:], g.pp.v(g.pp.ap[l]))
        P.dma("sp", bvt[:], g.bv.v(g.bv.ap[l, BV_LW:BV_LW + 2048].partition_broadcast(128)))
        P.ts(omk[:], pp[:, 121:129], -1.0, ALU.mult, 1.0, ALU.add)
        for q, cc in enumerate((C_MS, C_MI, C_MS, C_MI)):
            P.copy(mk4[:, q * 128:(q + 1) * 128], g.cst[:, cc:cc + 128])
        P.dma("pool", wa_up.v(wa_up.h[0:64, :]), V(g.w_up.ap[l], []))
        P.dma("pool", wa_up.v(wa_up.h[64:128, :]), V(g.a_up.ap[l], []))
        P.dma("pool", gA_up[:], V(g.g_up.ap[l, 0:128, :], []))
        P.dma("pool", gB_up.v(gB_up.h[0:32, :]), V(g.g_up.ap[l, 128:160, :], []))
        if l > 0:
            P.dma("pool", gB_up.v(gB_up.h[32:64, :]), V(g.v_up.ap[l - 1], []))
        with contextlib.ExitStack() as st1:
            A = P.sb(st1, "r_A0", [128, TT], F32)
            P.dma("sp", A[:], g.pT_d.v(g.pT_d.ap[3072:3200, :], 24))
            P.activation(waT.v(waT.h[0:64, :]), A.v(A.h[0:64, :]), AF.Tanh)
            P.copy(waT.v(waT.h[64:128, :]), A.v(A.h[64:128, :]))
            P.dma("sp", A[:], g.pT_d.v(g.pT_d.ap[3200:3328, :], 25))
            P.activation(gAT[:], A[:], AF.Sigmoid)
            nr = 64 if l > 0 else 32
            P.dma("sp", A.v(A.h[0:nr, :]), g.pT_d.v(g.pT_d.ap[3328:3328 + nr, :], 26))
            P.activation(gBT.v(gBT.h[0:32, :]), A.v(A.h[0:32, :]), AF.Sigmoid)
            if l > 0:
                P.copy(gBT.v(gBT.h[32:64, :]), A.v(A.h[32:64, :]))
            P.barrier()

        HS = 1024
        for hp in range(8):
            cs = slice(hp * 128, (hp + 1) * 128)
            with contextlib.ExitStack() as st2:
                t_r = P.sb(st2, "r_tr", [128, TT], F32)
                t_k = P.sb(st2, "r_tk", [128, TT], F32)
                t_v = P.sb(st2, "r_tv", [128, TT], F32)
                A = P.sb(st2, "r_A", [128, TT], F32)
                B = P.sb(st2, "r_B", [128, TT], F32)
                C = P.sb(st2, "r_C", [128, TT], F32)
                Dd = P.sb(st2, "r_D", [128, TT], F32)
                E = P.sb(st2, "r_E", [128, TT], F32)
                F = P.sb(st2, "r_F", [128, TT], F32)
                P.dma("sp", t_r[:], g.pT_d.v(g.pT_d.ap[hp * 128:(hp + 1) * 128, :], hp))
                P.dma("sp", t_k[:], g.pT_d.v(g.pT_d.ap[1024 + hp * 128:1024 + (hp + 1) * 128, :], 8 + hp))
                P.dma("sp", t_v[:], g.pT_d.v(g.pT_d.ap[2048 + hp * 128:2048 + (hp + 1) * 128, :], 16 + hp))

                def lora(dst, up, rows, src, bias_col):
                    for hf in range(2):
                        for tq in range(2):
                            t0 = hf * HS + tq * 512
                            P.mm(psL(slice(tq * 512, (tq + 1) * 512)), up.v(up.h[rows, cs]), src.v(src.h[rows, t0:t0 + 512]))
                        P.activation(dst[:, hf * HS:(hf + 1) * HS], psL(), AF.Sigmoid, bias=pp[:, bias_col:bias_col + 1])

                lora(A, wa_up, slice(0, 64), waT, 89 + hp)
                P.ts(A[:], A[:], -0.6065306597126334, ALU.mult)
                P.op("dve", lambda v: v.tensor_tensor_scan(B.h[:], rmask.h[:], A.h[:], 0.0, ALU.mult, ALU.add),
                     reads=[rmask[:], A[:]], writes=[B[:]])
                P.tt(A[:], B[:], A[:], ALU.subtract)
                P.activation(A[:], A[:], AF.Exp)
                P.activation(C[:], B[:], AF.Exp)
                P.activation(Dd[:], B[:], AF.Exp, scale=-1.0)
                P.copy(wc.v(wc.h[:].unsqueeze(2)), C.v(C.h[:].rearrange("p (c l) -> p c l", l=128)[:, :, 127:128]))
                lora(B, wa_up, slice(64, 128), waT, 97 + hp)
                if l > 0:
                    lora(E, gB_up, slice(32, 64), gBT, 105 + hp)
                    P.dma("sp", F[:], g.vfT_d.v(g.vfT_d.ap[cs, :], hp))
                    P.tt(F[:], F[:], t_v[:], ALU.subtract)
                    P.tt(F[:], F[:], E[:], ALU.mult)
                    P.tt(t_v[:], t_v[:], F[:], ALU.add)
                else:
                    P.dma("sp", g.vfT_d.v(g.vfT_d.ap[cs, :], hp), t_v[:])
                P.ts(E[:], t_k[:], pp[:, 113 + hp:114 + hp], ALU.mult)
                P.tt(F[:], E[:], E[:], ALU.mult)
                for hf in range(2):
                    for tq in range(2):
                        t0 = hf * HS + tq * 512
                        P.mm(psL(slice(tq * 512, (tq + 1) * 512)), g.cst[:, C_BO:C_BO + 128], F[:, t0:t0 + 512])
                    P.ts(F[:, hf * HS:(hf + 1) * HS], psL(), 1e-24, ALU.max)
                P.activation(F[:], F[:], AF.Ln)
                P.activation(F[:], F[:], AF.Exp, scale=-0.5)
                P.tt(E[:], E[:], F[:], ALU.mult)
                P.ts(F[:], B[:], pp[:, 121 + hp:122 + hp], ALU.mult, omk[:, hp:hp + 1], ALU.add)
                P.tt(F[:], t_k[:], F[:], ALU.mult)
                P.tt(t_k[:], t_r[:], F[:], ALU.mult)
                P.ts(t_k[:], t_k[:], pp[:, 129 + hp:130 + hp], ALU.mult)
                for tt_ in range(16):
                    P.mm(psL(slice(tt_ * 2, tt_ * 2 + 2)), t_k[:, tt_ * 128:(tt_ + 1) * 128], g.cst[:, C_S2:C_S2 + 2])
                P.copy(coef.v(coef.h[:].rearrange("p c h -> p (c h)")), psL(slice(0, 32)))
                a3 = lambda t: t.h[:].rearrange("p (c l) -> p c l", l=128)
                P.stt(arT.v(arT.h[:, :, 0, :]), E.v(a3(E)), -1.0, A.v(a3(A)), ALU.mult, ALU.mult)
                P.tt(arT.v(arT.h[:, :, 1, :]), t_r.v(a3(t_r)), C.v(a3(C)), ALU.mult)
                P.tt(B[:], E[:], B[:], ALU.mult)
                P.tt(bTb[:], B[:], Dd[:], ALU.mult)
                P.tt(kTb[:], F[:], Dd[:], ALU.mult)
                for (src, dst) in ((bTb, Btok), (kTb, Ktok)):
                    for half in range(2):
                        for j in range(8):
                            t_ = half * 8 + j
                            P.tr(pTb[:, j * 128:(j + 1) * 128], src[:, t_ * 128:(t_ + 1) * 128], g.identb[:], sig=(j == 7))
                        P.copy(dst.v(dst.h[:, half * 8:(half + 1) * 8, :].rearrange("p c t -> p (c t)")), pTb[:], e="act")
                for half in range(2):
                    for j in range(8):
                        t_ = half * 8 + j
                        P.tr(psL(slice(j * 128, (j + 1) * 128)), t_v[:, t_ * 128:(t_ + 1) * 128], g.ident, sig=(j == 7))
                    P.copy(Vtok.v(Vtok.h[:, half * 8:(half + 1) * 8, :].rearrange("p c t -> p (c t)")), psL())
                P.copy(Vbf[:], Vtok[:], e="act")
                P.barrier()

            with contextlib.ExitStack() as st3:
                A4a = P.sb(st3, "r_A4a", [128, 16, 2, 512], BF16)
                XTa = P.sb(st3, "r_XTa", [128, 16, 2, 128], BF16)
                Pl = [[P.sb(st3, f"r_Pl{u}{i}", [128, 2, 2, 128], BF16) for i in range(2)] for u in range(U)]
                XTf = [P.sb(st3, f"r_XTf{u}", [128, 2, 128], F32) for u in range(U)]
                Hf = P.sb(st3, "r_Hf", [128, 64], F32)
                tmpH = P.sb(st3, "r_tmpH", [128, 64], F32)
                Hblk = P.sb(st3, "r_Hblk", [128, 128], BF16)
                RHSb = P.sb(st3, "r_RHSb", [128, 128], BF16)
                Ub = [P.sb(st3, f"r_Ub{i}", [128, 128], BF16) for i in range(2)]
                ysb = [P.sb(st3, f"r_ysb{i}", [128, 128], F32) for i in range(2)]
                ysq = P.sb(st3, "r_ysq", [128, 128], F32)
                yn = P.sb(st3, "r_yn", [128, 128], F32)
                yo = P.sb(st3, "r_yo", [128, 128], BF16)
                stt_ = P.sb(st3, "r_st", [128, 12], F32)
                A4 = lambda c, sl=slice(0, 512), h=None: (A4a.k(("c", c), A4a.h[:, c, :, sl]) if h is None
                                                           else A4a.k(("c", c), A4a.h[:, c, h, sl]))
                XT = lambda c, h=None: (XTa.k(("c", c), XTa.h[:, c, :, :]) if h is None else XTa.k(("c", c), XTa.h[:, c, h, :]))
                mL = g.cst[:, C_ML:C_ML + 128]
                idb = g.cst.v(g.cst.h[:, C_ID:C_ID + 128].unsqueeze(1).to_broadcast([128, 2, 128]))

                def pLv(u):
                    qi, kk_ = u // 2, u % 2
                    return lambda fn: qv(qi, lambda h: fn(h[:, kk_ * 512:(kk_ + 1) * 512].rearrange("p (h a b) -> p h a b", h=2, a=2)), [kk_])

                def pX(u):
                    kk_, off = u // 2, (u % 2) * 256
                    return lambda fn: qv(2, lambda h: fn(h[:, kk_ * 512 + off:kk_ * 512 + off + 256].rearrange("p (h b) -> p h b", h=2)), [kk_])

                for g0 in range(0, 16, U):
                    units = list(range(g0, g0 + U))
                    for u, c in enumerate(units):
                        tsl = slice(c * 128, (c + 1) * 128)
                        for h in range(2):
                            pr = slice(h * 64, (h + 1) * 64)
                            ar = arT.v(arT.h[pr, c, :, :].rearrange("p a b -> p (a b)"))
                            P.mm(qv(0, lambda hh: hh[:, h * 512:h * 512 + 256], [h]), bTb[pr, tsl], ar)
                            P.mm(qv(0, lambda hh: hh[:, h * 512 + 256:h * 512 + 512], [h]), kTb[pr, tsl], ar)
                            P.mm(qv(1, lambda hh: hh[:, h * 512:h * 512 + 128], [h]), arT.v(arT.h[pr, c, 0, :]), bTb[pr, tsl])
                        for h in range(2):
                            P.tt(A4(c, h=h), qv(0, lambda hh: hh[:, h * 512:(h + 1) * 512], [h]), mk4[:], ALU.mult)
                            P.tt(Pl[u][0].v(Pl[u][0].h[:, h, 0, :]), qv(1, lambda hh: hh[:, h * 512:h * 512 + 128], [h]), mL, ALU.mult)
                        P.copy(Pl[u][0].v(Pl[u][0].h[:, :, 1, :]), A4(c, slice(0, 128)), e="act")
                        P.tt(XTf[u][:], A4(c, slice(0, 128)), idb, ALU.add)
                        P.copy(XT(c), XTf[u][:], e="act")
                    for k in range(1, 7):
                        for u, c in enumerate(units):
                            cur = Pl[u][(k - 1) % 2]
                            for h in range(2):
                                P.mm(pLv(u)(lambda a: a[:, h, 0, :]), cur.v(cur.h[:, h, 1, :]), cur.v(cur.h[:, h, 0, :]))
                                if k < 6:
                                    P.mm(pLv(u)(lambda a: a[:, h, 1, :]), cur.v(cur.h[:, h, 0, :]), cur.v(cur.h[:, h, 1, :]))
                        for u, c in enumerate(units):
                            nxt = Pl[u][k % 2]
                            if k < 6:
                                P.copy(nxt[:], pLv(u)(lambda a: a), e="act")
                            else:
                                P.copy(nxt.v(nxt.h[:, :, 0, :]), pLv(u)(lambda a: a[:, :, 0, :]), e="act")
                        for u, c in enumerate(units):
                            nxt = Pl[u][k % 2]
                            for h in range(2):
                                P.mm(pX(u)(lambda a: a[:, h, :]), nxt.v(nxt.h[:, h, 0, :]), XT(c, h))
                        for u, c in enumerate(units):
                            P.tt(XTf[u][:], XTf[u][:], pX(u)(lambda a: a), ALU.add)
                        for u, c in enumerate(units):
                            P.copy(XT(c), XTf[u][:], e="act")

                P.memset(Hf[:], 0.0)
                P.memset(Hblk[:], 0.0)
                pS = lambda sl: B6.v(B6.h[:, sl])

                def chain(c):
                    UB = Ub[c % 2]
                    P.mm(pS(slice(0, 128)), arT.v(arT.h[:, c, 0, :]), Hblk[:], start=True, stop=False)
                    for h in range(2):
                        hs = slice(h * 64, (h + 1) * 64)
                        P.mm(pS(slice(h * 64, (h + 1) * 64)), A4(c, slice(256, 384), h), Vbf.v(Vbf.h[:, c, hs]), start=False, stop=(h == 1))
                    P.copy(RHSb[:], pS(slice(0, 128)))
                    for h in range(2):
                        hs = slice(h * 64, (h + 1) * 64)
                        P.mm(pS(slice(128 + h * 64, 128 + (h + 1) * 64)), XT(c, h), RHSb[:, hs])
                    P.copy(UB[:], pS(slice(128, 256)), e="act")
                    P.mm(pS(slice(256, 384)), arT.v(arT.h[:, c, 1, :]), Hblk[:], start=True, stop=False)
                    for h in range(2):
                        hs = slice(h * 64, (h + 1) * 64)
                        P.mm(pS(slice(256 + h * 64, 256 + (h + 1) * 64)), A4(c, slice(128, 256), h), UB[:, hs], start=False, stop=False)
                        P.mm(pS(slice(256 + h * 64, 256 + (h + 1) * 64)), A4(c, slice(384, 512), h), Vbf.v(Vbf.h[:, c, hs]), start=False, stop=(h == 1))
                    P.mm(pS(slice(384, 512)), Btok.v(Btok.h[:, c, :]), UB[:], start=True, stop=False)
                    P.mm(pS(slice(384, 512)), Ktok.v(Ktok.h[:, c, :]), Vbf.v(Vbf.h[:, c, :]), start=False, stop=True)
                    for h in range(2):
                        pr = slice(h * 64, (h + 1) * 64)
                        P.ts(tmpH[pr, :], Hf[pr, :], wc[pr, c:c + 1], ALU.mult)
                        P.stt(Hf[pr, :], B6.v(B6.h[pr, 384 + h * 64:384 + (h + 1) * 64]),
                              wc[pr, c:c + 1], tmpH[pr, :], ALU.mult, ALU.add)
                        P.copy(Hblk[pr, h * 64:(h + 1) * 64], Hf[pr, :], e="act")
                    P.copy(ysb[c % 2][:], pS(slice(256, 384)), e="act")

                def epi(c):
                    tsl = slice(c * 128, (c + 1) * 128)
                    Y = ysb[c % 2]
                    y3 = Y.h[:].rearrange("p (h i) -> p h i", i=64)
                    P.op("dve", lambda v: v.tensor_reduce(stt_.h[:, 0:2], y3, AX.X, ALU.add), reads=[Y[:]], writes=[stt_[:]])
                    P.tt(ysq[:], Y[:], Y[:], ALU.mult)
                    q3 = ysq.h[:].rearrange("p (h i) -> p h i", i=64)
                    P.op("dve", lambda v: v.tensor_reduce(stt_.h[:, 2:4], q3, AX.X, ALU.add), reads=[ysq[:], stt_[:]], writes=[stt_[:]])
                    P.ts(stt_[:, 4:8], stt_[:, 0:4], 1.0 / 64.0, ALU.mult)
                    P.tt(stt_[:, 8:10], stt_[:, 4:6], stt_[:, 4:6], ALU.mult)
                    P.tt(stt_[:, 8:10], stt_[:, 6:8], stt_[:, 8:10], ALU.subtract)
                    P.activation(stt_[:, 8:10], stt_[:, 8:10], AF.Sqrt, bias=64e-5)
                    P.op("dve", lambda v: v.reciprocal(stt_.h[:, 8:10], stt_.h[:, 8:10]), reads=[stt_[:]], writes=[stt_[:]])
                    P.tt(stt_[:, 10:12], stt_[:, 4:6], stt_[:, 8:10], ALU.mult)
                    for h in range(2):
                        hs = slice(h * 64, (h + 1) * 64)
                        P.ts(yn[:, hs], Y[:, hs], stt_[:, 8 + h:9 + h], ALU.mult, stt_[:, 10 + h:11 + h], ALU.subtract)
                    P.tt(yn[:], yn[:], bvt[:, hp * 128:(hp + 1) * 128], ALU.mult)
                    P.tt(yn[:], yn[:], bvt[:, 1024 + hp * 128:1024 + (hp + 1) * 128], ALU.add)
                    for h in range(2):
                        hs = slice(h * 64, (h + 1) * 64)
                        P.stt(yn[:, hs], Vtok.v(Vtok.h[:, c, hs]), coef.v(coef.h[:, c, h:h + 1]), yn[:, hs], ALU.mult, ALU.add)
                    gps = qv(2, lambda hh: hh[:, 0:128], [0])
                    P.mm(gps, gAT[:, tsl], gA_up[:, cs], start=True, stop=False)
                    P.mm(gps, gBT.v(gBT.h[0:32, tsl]), gB_up.v(gB_up.h[0:32, cs]), start=False, stop=True)
                    P.tt(yo[:], yn[:], gps, ALU.mult)
                    P.tr(pTb[:, 0:128], yo[:], g.identb[:])
                    P.copy(yrwT[:, tsl], pTb[:, 0:128], e="act")

                chain(0)
                for c in range(1, 16):
                    chain(c)
                    epi(c - 1)
                epi(15)
                P.dma("sp", g.yT_d.v(g.yT_d.ap[1024 + hp * 128:1024 + (hp + 1) * 128, :], "rw%d" % hp), yrwT[:])
                P.barrier()


def ln_tiles(P, st, pfx, nt=1):
    d = Ctx()
    d.ts_ = [P.sb(st, pfx + f"_t{i}", [128, D], F32) for i in range(nt)]
    d.t = d.ts_[0]
    d.xb = P.sb(st, pfx + "_xb", [128, D], BF16)
    d.xo = P.sb(st, pfx + "_xo", [128, 16, 128], BF16)
    d.bst = P.sb(st, pfx + "_bst", [128, 4, 6], F32)
    d.mv = P.sb(st, pfx + "_mv", [128, 2], F32)
    d.pT = P.ps(st, pfx + "_pT", [128, 1024], BF16)
    return d


def ln_epilogue_gen(g, P, d, t, tt, gb, dst, write_xT):
    for q in range(4):
        P.op("dve", lambda v, q=q: v.bn_stats(d.bst.h[:, q, :], t.h[:, q * 512:(q + 1) * 512]), reads=[t[:]], writes=[d.bst[:]])
    P.op("dve", lambda v: v.bn_aggr(d.mv.h[:], d.bst.h[:].rearrange("p a b -> p (a b)")), reads=[d.bst[:]], writes=[d.mv[:]])
    P.activation(d.mv[:, 1:2], d.mv[:, 1:2], AF.Sqrt, bias=1e-5)
    P.op("dve", lambda v: v.reciprocal(d.mv.h[:, 1:2], d.mv.h[:, 1:2]), reads=[d.mv[:]], writes=[d.mv[:]])
    yield
    P.ts(t[:], t[:], d.mv[:, 0:1], ALU.subtract, d.mv[:, 1:2], ALU.mult)
    yield
    P.tt(t[:], t[:], gb[0], ALU.mult)
    yield
    P.tt(t[:], t[:], gb[1], ALU.add)
    rows = slice(tt * 128, (tt + 1) * 128)
    P.dma("sp", dst.v(dst.ap[rows, :], tt), t[:])
    if write_xT:
        P.copy(d.xb[:], t[:], e="act")
        yield
        for half in range(2):
            for j in range(8):
                c = half * 8 + j
                P.tr(d.pT[:, j * 128:(j + 1) * 128], d.xb[:, c * 128:(c + 1) * 128], g.identb[:], sig=(j == 7))
            P.copy(d.xo.v(d.xo.h[:, half * 8:(half + 1) * 8, :].rearrange("p c t -> p (c t)")), d.pT[:], e="act")
        P.dma("sp", g.xT_d.v(g.xT_d.ap.rearrange("(c p) t -> p c t", p=128)[:, :, rows], tt), d.xo[:])
    yield


def run_gen(gen):
    for _ in gen:
        pass


def phase_wout_ln(g, l):
    P = g.P
    xsrc = g.x if l == 0 else g.xres
    with contextlib.ExitStack() as st:
        wo = P.sb(st, "o_wo", [128, 16, D], BF16)
        bvt = P.sb(st, "o_bv", [128, 4096], F32)
        yq = [P.sb(st, f"o_yq{i}", [128, 16, 512], BF16) for i in range(2)]
        xr = [P.sb(st, f"o_xr{i}", [128, D], F32) for i in range(2)]
        ps = P.ps(st, "o_ps", [128, D], F32)
        d = ln_tiles(P, st, "o", nt=2)
        src = g.w_out.ap[l].rearrange("(c p) m -> p c m", p=128)
        for kh in range(4):
            for ch in range(2):
                P.dma("pool", wo.v(wo.h[:, kh * 4:(kh + 1) * 4, ch * 1024:(ch + 1) * 1024]),
                      V(src[:, kh * 4:(kh + 1) * 4, ch * 1024:(ch + 1) * 1024], []))
        P.dma("sp", bvt[:], g.bv.v(g.bv.ap[l, BV_G1:BV_G1 + 4096].partition_broadcast(128)))
        yv = g.yT_d.ap.rearrange("(c p) t -> p c t", p=128)

        def ldq(q):
            P.dma("sp", yq[q % 2][:], g.yT_d.v(yv[:, :, q * 512:(q + 1) * 512], "ssd", *["rw%d" % i for i in range(8)]))

        def mm_tile(tt):
            q, j = tt // 4, tt % 4
            if j == 0 and q + 1 < 4:
                ldq(q + 1)
            rows = slice(tt * 128, (tt + 1) * 128)
            P.dma("sp", xr[tt % 2][:], xsrc.v(xsrc.ap[rows, :], tt))
            Y = yq[q % 2]
            for db in range(4):
                for kc in range(16):
                    P.mm(ps[:, db * 512:(db + 1) * 512], Y.v(Y.h[:, kc, j * 128:(j + 1) * 128]), wo.v(wo.h[:, kc, db * 512:(db + 1) * 512]),
                         start=(kc == 0), stop=(kc == 15))
        ldq(0)
        mm_tile(0)
        for tt in range(16):
            t = d.ts_[tt % 2]
            P.stt(t[:], xr[tt % 2][:], ALPHA, ps[:], ALU.mult, ALU.add)
            if tt + 1 < 16:
                mm_tile(tt + 1)
            run_gen(ln_epilogue_gen(g, P, d, t, tt, (bvt[:, 0:2048], bvt[:, 2048:4096]), g.xres, True))
        P.barrier()


def phase_ffn(g, l, last):
    P = g.P
    moe = (l % 2 == 1)
    li = l // 2
    if moe:
        groups = [(g.moe_w1.ap[li, e], g.moe_w3.ap[li, e], g.moe_w2.ap[li, e]) for e in range(8)]
    else:
        groups = [(g.ffn_w1.ap[li][:, gi * DFE:(gi + 1) * DFE], g.ffn_w3.ap[li][:, gi * DFE:(gi + 1) * DFE],
                   g.ffn_w2.ap[li][gi * DFE:(gi + 1) * DFE, :]) for gi in range(2)]
    dst = g.out if last else g.xres
    NFB = 22
    with contextlib.ExitStack() as st:
        bvt = P.sb(st, "f_bv", [128, 4096], F32)
        xTq = P.sb(st, "f_xTq", [128, 16, 512], BF16)
        x1 = [P.sb(st, f"f_x1{i}", [128, D], F32) for i in range(2)]
        acc = P.sb(st, "f_acc", [128, 4, D], F32)
        w13 = [[P.sb(st, f"f_w{a}{i}", [128, 16, 128], BF16) for i in range(2)] for a in (1, 3)]
        hT = P.sb(st, "f_hT", [128, NFB, 512], BF16)
        w2t = [P.sb(st, f"f_w2{i}", [128, 11, 256], BF16) for i in range(4)]
        gact = [P.sb(st, f"f_ga{i}", [128, 512], F32) for i in range(2)]
        ps13 = [[P.ps(st, f"f_ps{a}{i}", [128, 512], F32) for i in range(2)] for a in (1, 3)]
        psw2 = [P.ps(st, f"f_psw{i}", [128, 2, 256], F32) for i in range(2)]
        d = ln_tiles(P, st, "f")
        x1e = P.sb(st, "f_x1e", [128, D], F32)
        if moe:
            junk = P.sb(st, "f_junk", [128, D], F32)
            rt = P.sb(st, "f_rt", [128, D], F32)
            lg = P.sb(st, "f_lg", [128, 4, 8], F32)
            comb = P.sb(st, "f_comb", [128, 4, 8], F32)
            m8 = P.sb(st, "f_m8", [128, 8], F32)
            gg = P.sb(st, "f_gg", [128, 2], F32)
            eq = P.sb(st, "f_eq", [128, 8], F32)
        P.dma("sp", bvt[:], g.bv.v(g.bv.ap[l, BV_G2:BV_G2 + 4096].partition_broadcast(128)))
        xv = g.xT_d.ap.rearrange("(c p) t -> p c t", p=128)
        def router_gen(q):
            for e in range(8):
                P.dma("sp", rt[:], g.routerT.v(g.routerT.ap[li, e].partition_broadcast(128)))
                for j in range(4):
                    tt = q * 4 + j
                    P.dma("sp", x1[j % 2][:], g.xres.v(g.xres.ap[tt * 128:(tt + 1) * 128, :], tt))
                    P.stt(junk[:], x1[j % 2][:], 1.0, rt[:], ALU.mult, ALU.mult, accum_out=lg.v(lg.h[:, j, e:e + 1]))
                    yield
            for j in range(4):
                P.op("dve", lambda v, j=j: v.max(m8.h[:], lg.h[:, j, :]), reads=[lg[:]], writes=[m8[:]])
                P.tt(gg[:, 0:1], m8[:, 0:1], m8[:, 1:2], ALU.subtract)
                P.activation(gg[:, 0:1], gg[:, 0:1], AF.Sigmoid)
                P.ts(gg[:, 1:2], gg[:, 0:1], -1.0, ALU.mult, 1.0, ALU.add)
                P.ts(eq[:], lg.v(lg.h[:, j, :]), m8[:, 0:1], ALU.is_equal)
                P.ts(comb.v(comb.h[:, j, :]), eq[:], gg[:, 0:1], ALU.mult)
                P.ts(eq[:], lg.v(lg.h[:, j, :]), m8[:, 1:2], ALU.is_equal)
                P.stt(comb.v(comb.h[:, j, :]), eq[:], gg[:, 1:2], comb.v(comb.h[:, j, :]), ALU.mult, ALU.add)
                yield

        def epi_gen(q):
            for j in range(4):
                tt = q * 4 + j
                t = d.ts_[0]
                P.dma("sp", x1e[:], g.xres.v(g.xres.ap[tt * 128:(tt + 1) * 128, :], tt))
                P.stt(t[:], x1e[:], ALPHA, acc.v(acc.h[:, j, :]), ALU.mult, ALU.add)
                yield
                yield from ln_epilogue_gen(g, P, d, t, tt, (bvt[:, 0:2048], bvt[:, 2048:4096]), dst, not last)

        import itertools
        pending = iter(())
        for q in range(4):
            P.dma("sp", xTq[:], g.xT_d.v(xv[:, :, q * 512:(q + 1) * 512], *range(q * 4, q * 4 + 4)))
            if moe:
                pending = itertools.chain(pending, router_gen(q))
            for gi, (w1a, w3a, w2a) in enumerate(groups):
                w1v = w1a.rearrange("(c p) m -> p c m", p=128)
                w3v = w3a.rearrange("(c p) m -> p c m", p=128)
                w2v = w2a.rearrange("(f p) m -> p f m", p=128)

                def ldw(fb):
                    P.dma("pool", w13[0][fb % 2][:], V(w1v[:, :, fb * 128:(fb + 1) * 128], []))
                    P.dma("pool", w13[1][fb % 2][:], V(w3v[:, :, fb * 128:(fb + 1) * 128], []))
                ldw(0)
                for fb in range(NFB):
                    if fb + 1 < NFB:
                        ldw(fb + 1)
                    i = fb % 2
                    for a in range(2):
                        for kc in range(16):
                            P.mm(ps13[a][i][:], w13[a][i].v(w13[a][i].h[:, kc, :]), xTq.v(xTq.h[:, kc, :]),
                                 start=(kc == 0), stop=(kc == 15))
                    P.activation(gact[i][:], ps13[0][i][:], AF.Silu)
                    P.tt(hT.v(hT.h[:, fb, :]), gact[i][:], ps13[1][i][:], ALU.mult)
                    if gi == 0:
                        for _ in range(3):
                            next(pending, None)
                if gi == 0:
                    run_gen(pending)
                    pending = iter(())
                nld = 0
                for dp in range(8):
                    cols = slice(dp * 256, (dp + 1) * 256)
                    wts = []
                    for hf in range(2):
                        wtile = w2t[nld % 4]
                        nld += 1
                        P.dma("pool", wtile[:], V(w2v[:, hf * 11:(hf + 1) * 11, cols], []))
                        wts.append(wtile)
                    for jh in range(2):
                        psw = psw2[jh]
                        for j2 in range(2):
                            j = jh * 2 + j2
                            for fb in range(NFB):
                                wtile = wts[fb // 11]
                                P.mm(psw.v(psw.h[:, j2, :]), hT.v(hT.h[:, fb, j * 128:(j + 1) * 128]), wtile.v(wtile.h[:, fb % 11, :]),
                                     start=(fb == 0), stop=(fb == NFB - 1))
                        a3 = acc.v(acc.h[:, jh * 2:jh * 2 + 2, cols])
                        if moe:
                            for j2 in range(2):
                                j = jh * 2 + j2
                                aj = acc.v(acc.h[:, j, cols])
                                if gi == 0:
                                    P.ts(aj, psw.v(psw.h[:, j2, :]), comb.v(comb.h[:, j, gi:gi + 1]), ALU.mult)
                                else:
                                    P.stt(aj, psw.v(psw.h[:, j2, :]), comb.v(comb.h[:, j, gi:gi + 1]), aj, ALU.mult, ALU.add)
                        else:
                            if gi == 0:
                                P.copy(a3, psw[:])
                            else:
                                P.tt(a3, a3, psw[:], ALU.add)
            pending = epi_gen(q)
        run_gen(pending)
        P.barrier()


def make_consts():
    c = np.zeros((128, CW), np.float32)
    r = np.arange(128)[:, None]
    q = np.arange(128)[None, :]
    c[:, C_ID:C_ID + 128] = (r == q)
    c[:, C_MI:C_MI + 128] = (q >= r)
    c[:, C_MS:C_MS + 128] = (q > r)
    c[:, C_ML:C_ML + 128] = (q < r)
    c[:, C_BO:C_BO + 128] = ((r // 64) == (q // 64))
    c[:, C_S2:C_S2 + 2] = ((r // 64) == np.arange(2)[None, :])
    s16 = np.zeros((128, 16, 128), np.float32)
    for e in range(16):
        s16[e, e, :] = 1.0
    c[:, C_S16:C_S16 + 2048] = s16.reshape(128, 2048)
    rm = np.ones((128, TT), np.float32)
    rm[:, ::128] = 0.0
    c[:, C_RM:C_RM + TT] = rm
    return c


def pack_params(inp):
    pp = np.zeros((NL, 128, NPP), np.float32)
    bv = np.zeros((NL, NBV), np.float32)

    def cols(vec):
        return np.ascontiguousarray(vec.reshape(-1, 128).T)

    for l in range(NL):
        cw = inp["ssd_conv_w"][l]
        pp[l, :, 0:48] = cw.reshape(4, 12, 128).transpose(2, 1, 0).reshape(128, 48)
        pp[l, :, 48:60] = cols(inp["ssd_conv_b"][l])
        pp[l, 0:16, 60] = inp["ssd_dt_bias"][l]
        pp[l, 0:16, 61] = inp["ssd_a_log"][l]
        mix = inp["rw_mix"][l]
        pp[l, :, 62:86] = cols(mix[0:3072])
        pp[l, :, 86] = mix[3072:3200]
        pp[l, :, 87] = mix[3200:3328]
        pp[l, 0:32, 88] = mix[3328:3360]
        if l > 0:
            pp[l, 32:64, 88] = inp["rw_vres_mix"][l - 1]
            pp[l, :, 105:113] = cols(inp["rw_v0"][l - 1])
        pp[l, :, 89:97] = cols(inp["rw_w0"][l])
        pp[l, :, 97:105] = cols(inp["rw_a0"][l])
        pp[l, :, 113:121] = cols(inp["rw_k_k"][l])
        pp[l, :, 121:129] = cols(inp["rw_k_a"][l])
        pp[l, :, 129:137] = cols(inp["rw_r_k"][l].reshape(-1))
        bv[l, BV_D:BV_D + 16] = inp["ssd_d"][l]
        bv[l, BV_NW:BV_NW + 1024] = inp["ssd_norm_w"][l]
        bv[l, BV_LW:BV_LW + 1024] = inp["rw_ln_w"][l]
        bv[l, BV_LB:BV_LB + 1024] = inp["rw_ln_b"][l]
        bv[l, BV_G1:BV_G1 + 2048] = inp["ln1_g"][l]
        bv[l, BV_B1:BV_B1 + 2048] = inp["ln1_b"][l]
        bv[l, BV_G2:BV_G2 + 2048] = inp["ln2_g"][l]
        bv[l, BV_B2:BV_B2 + 2048] = inp["ln2_b"][l]
    return pp, bv


def make_in_maps(inp, cores):
    f = lambda a: np.ascontiguousarray(np.asarray(a, dtype=np.float32))
    pp, bv = pack_params({k: np.asarray(v) for k, v in inp.items()})
    shared = {
        "w_in": f(inp["w_in"]), "w_in_vres": f(inp["w_in_vres"]), "rw_w_up": f(inp["rw_w_up"]),
        "rw_a_up": f(inp["rw_a_up"]), "rw_v_up": f(inp["rw_v_up"]), "rw_g_up": f(inp["rw_g_up"]),
        "w_out": f(inp["w_out"]), "ffn_w1": f(inp["ffn_w1"]), "ffn_w3": f(inp["ffn_w3"]), "ffn_w2": f(inp["ffn_w2"]),
        "moe_w1": f(inp["moe_w1"]), "moe_w3": f(inp["moe_w3"]), "moe_w2": f(inp["moe_w2"]),
        "routerT": f(np.transpose(np.asarray(inp["moe_router"]), (0, 2, 1))),
        "pp": pp, "bv": bv, "consts": make_consts(),
    }
    x = np.asarray(inp["x"], dtype=np.float32)
    return [dict(shared, x=np.ascontiguousarray(x[c])) for c in cores]


def kernel(**inputs):
    nc = build_program()
    in_maps = make_in_maps(inputs, list(range(8)))
    res = run_bass_kernel_spmd(nc, in_maps, core_ids=list(range(8)))
    return np.stack([np.asarray(r["out"], dtype=np.float32) for r in res.results], axis=0)
```

```python
import contextlib
import numpy as np
import concourse.bass as bass
import concourse.mybir as mybir
from concourse.bass_utils import run_bass_kernel_spmd

F32 = mybir.dt.float32
BF16 = mybir.dt.bfloat16
AF = mybir.ActivationFunctionType
ALU = mybir.AluOpType
AX = mybir.AxisListType

TT = 2048
D = 2048
NL = 4
IN_DIM = 5936
DFF = 5632
DFE = 2816
ALPHA = float((2 * NL) ** 0.25)
NPP = 137
NBV = 11280
CW = 4738
BV_D, BV_NW, BV_LW, BV_LB, BV_G1, BV_B1, BV_G2, BV_B2 = 0, 16, 1040, 2064, 3088, 5136, 7184, 9232
C_ID, C_MI, C_MS, C_ML, C_BO, C_S2, C_S16, C_RM = 0, 128, 256, 384, 512, 640, 642, 2690


class Buf:
    __slots__ = ("name", "w", "r")

    def __init__(self, name):
        self.name = name
        self.w = []
        self.r = []


class V:
    __slots__ = ("ap", "bufs")

    def __init__(self, ap, bufs):
        self.ap = ap
        self.bufs = bufs


class T:
    def __init__(self, handle, name):
        self.h = handle
        self.buf = Buf(name)
        self.sub = {}

    def __getitem__(self, idx):
        return V(self.h[idx], [self.buf])

    def v(self, ap):
        return V(ap, [self.buf])

    def kb(self, key):
        b = self.sub.get(key)
        if b is None:
            b = self.sub[key] = Buf(f"{self.buf.name}.{key}")
        return b

    def k(self, keys, ap):
        if not isinstance(keys, (list, tuple)):
            keys = [keys]
        return V(ap, [self.kb(k) for k in keys])


class DT:
    def __init__(self, nc, name, shape, dtype, kind="Internal"):
        self.h = nc.dram_tensor(name, list(shape), dtype, kind=kind)
        self.ap = self.h.ap()
        self.name = name
        self.bufs = {}

    def v(self, ap, *keys):
        bs = []
        for key in (keys or (None,)):
            b = self.bufs.get(key)
            if b is None:
                b = self.bufs[key] = Buf(f"{self.name}.{key}")
            bs.append(b)
        return V(ap, bs)


class Prog:
    NDMA = 8

    def __init__(self, nc, stack):
        self.nc = nc
        self.stack = stack
        self.eng = {"pe": nc.tensor, "act": nc.scalar, "dve": nc.vector, "pool": nc.gpsimd, "sp": nc.sync}
        self.csem = {}
        self.ccount = {}
        for e in ("pe", "act", "dve", "pool"):
            self.csem[e] = stack.enter_context(nc.semaphore(f"c_{e}"))
            self.ccount[e] = 0
        self.dsem = {}
        self.dcount = {}
        for q in ("sp", "pool"):
            self.dsem[q] = [stack.enter_context(nc.semaphore(f"d_{q}{i}")) for i in range(self.NDMA)]
            self.dcount[q] = 0
        self.waited = {e: {} for e in self.eng}
        self.nops = 0

    def sb(self, st, name, shape, dtype):
        self.nalloc = getattr(self, "nalloc", 0) + 1
        name = f"{name}_{self.nalloc}"
        return T(st.enter_context(self.nc.sbuf_tensor(name, list(shape), dtype)), name)

    def ps(self, st, name, shape, dtype=F32):
        self.nalloc = getattr(self, "nalloc", 0) + 1
        name = f"{name}_{self.nalloc}"
        return T(st.enter_context(self.nc.psum_tensor(name, list(shape), dtype)), name)

    def _sem(self, key):
        kind, q, i = key
        return self.csem[q] if kind == "c" else self.dsem[q][i]

    def _wait(self, e, tokens):
        eng = self.eng[e]
        need = {}
        for (key, val) in tokens:
            if key[0] == "c" and key[1] == e and e == "pe":
                continue
            if self.waited[e].get(key, 0) >= val:
                continue
            if need.get(key, 0) < val:
                need[key] = val
        for key, val in need.items():
            eng.wait_ge(self._sem(key), val)
            self.waited[e][key] = val

    @staticmethod
    def _deps(reads, writes):
        toks = []
        for v in reads:
            for b in v.bufs:
                toks += b.w
        for v in writes:
            for b in v.bufs:
                toks += b.w
                toks += b.r
        return toks

    @staticmethod
    def _mark(reads, writes, tok):
        for v in reads:
            for b in v.bufs:
                for i_, (k_, v_) in enumerate(b.r):
                    if k_ == tok[0]:
                        if v_ < tok[1]:
                            b.r[i_] = tok
                        break
                else:
                    b.r.append(tok)
        for v in writes:
            for b in v.bufs:
                b.w = [tok]
                b.r = []

    def op(self, e, fn, reads=(), writes=(), sig=True):
        self._wait(e, self._deps(reads, writes))
        ins = fn(self.eng[e])
        self.nops += 1
        if sig:
            self.ccount[e] += 1
            ins.then_inc(self.csem[e], 1)
            tok = (("c", e, 0), self.ccount[e])
        else:
            tok = (("c", e, 0), self.ccount[e] + 1)
        self._mark(reads, writes, tok)
        return ins

    def dma(self, q, out, in_, **kw):
        k = self.dcount[q]
        self.dcount[q] += 1
        i = k % self.NDMA
        gen = k // self.NDMA
        key = ("d", q, i)
        toks = [(key, 16 * gen)] if gen > 0 else []
        toks += self._deps([in_], [out])
        self._wait(q, toks)
        ins = self.eng[q].dma_start(out=out.ap, in_=in_.ap, **kw)
        ins.then_inc(self.dsem[q][i], 16)
        self.nops += 1
        self._mark([in_], [out], (key, 16 * (gen + 1)))
        return ins

    def barrier(self):
        toks = [(("c", e, 0), self.ccount[e]) for e in self.csem if self.ccount[e] > 0]
        for q in self.dsem:
            k = self.dcount[q]
            for i in range(self.NDMA):
                n = (k - i + self.NDMA - 1) // self.NDMA if k > i else 0
                if n > 0:
                    toks.append((("d", q, i), 16 * n))
        for e in self.eng:
            self._wait(e, toks)

    def mm(self, out, lhsT, rhs, start=True, stop=True, sig=None):
        if sig is None:
            sig = stop
        return self.op("pe", lambda t: t.matmul(out.ap, lhsT.ap, rhs.ap, start=start, stop=stop),
                       reads=[lhsT, rhs], writes=[out], sig=sig)

    def tr(self, out, in_, ident, sig=True):
        return self.op("pe", lambda t: t.transpose(out.ap, in_.ap, ident.ap), reads=[in_, ident], writes=[out], sig=sig)

    def activation(self, out, in_, func, bias=None, scale=None, accum_out=None):
        reads = [in_]
        kw = {}
        if bias is not None:
            if isinstance(bias, V):
                reads.append(bias); kw["bias"] = bias.ap
            else:
                kw["bias"] = bias
        if scale is not None:
            if isinstance(scale, V):
                reads.append(scale); kw["scale"] = scale.ap
            else:
                kw["scale"] = scale
        writes = [out]
        if accum_out is not None:
            writes.append(accum_out); kw["accum_out"] = accum_out.ap
        return self.op("act", lambda s: s.activation(out.ap, in_.ap, func, **kw), reads=reads, writes=writes)

    def tt(self, out, in0, in1, op, e="dve"):
        return self.op(e, lambda v: v.tensor_tensor(out.ap, in0.ap, in1.ap, op), reads=[in0, in1], writes=[out])

    def ts(self, out, in0, s1, op0, s2=None, op1=None, e="dve"):
        reads = [in0]
        a1 = s1
        if isinstance(s1, V):
            reads.append(s1); a1 = s1.ap
        a2 = s2
        if isinstance(s2, V):
            reads.append(s2); a2 = s2.ap
        kw = {}
        if op1 is not None:
            kw["op1"] = op1
        return self.op(e, lambda v: v.tensor_scalar(out.ap, in0.ap, a1, a2, op0, **kw), reads=reads, writes=[out])

    def stt(self, out, in0, scalar, in1, op0, op1, accum_out=None):
        reads = [in0, in1]
        a = scalar
        if isinstance(scalar, V):
            reads.append(scalar); a = scalar.ap
        writes = [out]
        kw = {}
        if accum_out is not None:
            writes.append(accum_out); kw["accum_out"] = accum_out.ap
        return self.op("dve", lambda v: v.scalar_tensor_tensor(out.ap, in0.ap, a, in1.ap, op0, op1, **kw),
                       reads=reads, writes=writes)

    def copy(self, out, in_, e="dve"):
        if e == "act":
            return self.op(e, lambda s: s.copy(out.ap, in_.ap), reads=[in_], writes=[out])
        return self.op(e, lambda v: v.tensor_copy(out.ap, in_.ap), reads=[in_], writes=[out])

    def memset(self, out, val, e="dve"):
        return self.op(e, lambda v: v.memset(out.ap, val), reads=[], writes=[out])


def r3(ap, q):
    return ap.rearrange("p (e q) -> p e q", q=q)


def bc3(ap, n):
    return ap.unsqueeze(2).to_broadcast([ap.shape[0], ap.shape[1], n])


class Ctx:
    pass


def build_program(n_layers=NL, dbg=(), phases="isrof"):
    nc = bass.Bass("TRN2", target_bir_lowering=False)
    g = Ctx()
    g.nc = nc

    def din(name, shape):
        return DT(nc, name, shape, F32, kind="ExternalInput")

    def scratch(name, shape, dtype):
        return DT(nc, name, shape, dtype, kind=("ExternalOutput" if name in dbg else "Internal"))

    g.x = din("x", [TT, D])
    g.w_in = din("w_in", [NL, D, IN_DIM])
    g.w_vres = din("w_in_vres", [NL - 1, D, 32])
    g.w_up = din("rw_w_up", [NL, 64, 1024])
    g.a_up = din("rw_a_up", [NL, 64, 1024])
    g.v_up = din("rw_v_up", [NL - 1, 32, 1024])
    g.g_up = din("rw_g_up", [NL, 160, 1024])
    g.w_out = din("w_out", [NL, D, D])
    g.ffn_w1 = din("ffn_w1", [2, D, DFF])
    g.ffn_w3 = din("ffn_w3", [2, D, DFF])
    g.ffn_w2 = din("ffn_w2", [2, DFF, D])
    g.moe_w1 = din("moe_w1", [2, 8, D, DFE])
    g.moe_w3 = din("moe_w3", [2, 8, D, DFE])
    g.moe_w2 = din("moe_w2", [2, 8, DFE, D])
    g.routerT = din("routerT", [2, 8, D])
    g.pp = din("pp", [NL, 128, NPP])
    g.bv = din("bv", [NL, NBV])
    g.consts = din("consts", [128, CW])
    g.out = DT(nc, "out", [TT, D], F32, kind="ExternalOutput")

    g.xres = scratch("xres", [TT, D], F32)
    g.xT_d = scratch("xT_d", [D, TT], BF16)
    g.zs_d = scratch("zs_d", [TT, 1024], F32)
    g.xs_d = scratch("xs_d", [TT, 1024], F32)
    g.BT_d = scratch("BT_d", [256, TT], BF16)
    g.CT_d = scratch("CT_d", [256, TT], BF16)
    g.Btok_d = scratch("Btok_d", [TT, 256], BF16)
    g.dtT_d = scratch("dtT_d", [16, TT], F32)
    g.acT_d = scratch("acT_d", [16, TT], F32)
    g.dttok_d = scratch("dttok_d", [TT, 32], F32)
    g.pT_d = scratch("pT_d", [3456, TT], F32)
    g.vfT_d = scratch("vfT_d", [1024, TT], F32)
    g.yT_d = scratch("yT_d", [D, TT], BF16)
    g.comb_d = scratch("comb_d", [TT, 8], F32)
    g.c13 = scratch("c13", [2 * 8 * 22 * 128, 2048], BF16)
    g.c2 = scratch("c2", [8 * 8 * 2 * 128, 11 * 256], BF16)

    with contextlib.ExitStack() as st0:
        P = Prog(nc, st0)
        g.P = P
        g.cst = P.sb(st0, "cst", [128, C_S16], F32)
        g.identb = P.sb(st0, "identb", [128, 128], BF16)
        P.dma("sp", g.cst[:], g.consts.v(g.consts.ap[:, 0:C_S16]))
        P.copy(g.identb[:], g.cst[:, C_ID:C_ID + 128])
        g.ident = g.cst[:, C_ID:C_ID + 128]

        phase_ln_transpose_init(g)
        for l in range(n_layers):
            if "i" in phases:
                phase_inproj(g, l)
            if "s" in phases:
                phase_ssd(g, l)
            if "r" in phases:
                phase_rwkv(g, l)
            if "o" in phases:
                phase_wout_ln(g, l)
            if "f" in phases:
                phase_ffn(g, l, last=(l == n_layers - 1))
        P.barrier()
    return nc


def emit_transposes_bf16(g, P, src_bf, pst, dst_view_fn, nblk):
    for c in range(nblk):
        P.tr(pst[:, c * 128:(c + 1) * 128], src_bf[:, c * 128:(c + 1) * 128], g.identb[:], sig=(c == nblk - 1))


def phase_ln_transpose_init(g):
    P = g.P
    with contextlib.ExitStack() as st:
        xin = [P.sb(st, f"i_x{i}", [128, D], F32) for i in range(2)]
        xb = [P.sb(st, f"i_xb{i}", [128, D], BF16) for i in range(2)]
        xo = [P.sb(st, f"i_xo{i}", [128, 16, 128], BF16) for i in range(2)]
        pst = [P.ps(st, f"i_ps{i}", [128, D], BF16) for i in range(2)]
        for tt in range(16):
            i = tt % 2
            P.dma("sp", xin[i][:], g.x.v(g.x.ap[tt * 128:(tt + 1) * 128, :], tt))
            P.copy(xb[i][:], xin[i][:], e="act")
            emit_transposes_bf16(g, P, xb[i], pst[i], None, 16)
            P.copy(xo[i].v(xo[i].h[:].rearrange("p c t -> p (c t)")), pst[i][:])
            P.dma("sp", g.xT_d.v(g.xT_d.ap.rearrange("(c p) t -> p c t", p=128)[:, :, tt * 128:(tt + 1) * 128], tt), xo[i][:])
        P.barrier()


def load_wblock(P, wt, src_ap, ncols):
    s = src_ap.rearrange("(c p) m -> p c m", p=128)
    for hf in range(2):
        P.dma("pool", wt.v(wt.h[:, hf * 8:(hf + 1) * 8, 0:ncols]), V(s[:, hf * 8:(hf + 1) * 8, :], []))


def phase_inproj(g, l):
    P = g.P
    with contextlib.ExitStack() as st:
        xT = P.sb(st, "p_xT", [128, 16, TT], BF16)
        wt = [P.sb(st, f"p_w{i}", [128, 16, 512], BF16) for i in range(2)]
        pp = P.sb(st, "p_pp", [128, NPP], F32)
        bigh = [P.ps(st, f"p_big{i}", [128, 1024], F32) for i in range(2)]
        ptr = P.ps(st, "p_trf", [128, 1024], F32)
        pbt = P.ps(st, "p_trb", [128, 2048], BF16)
        ub = [P.sb(st, f"p_u{i}", [128, 4 + TT], F32) for i in range(2)]
        acc = P.sb(st, "p_acc", [128, TT], F32)
        ob = [P.sb(st, f"p_ob{i}", [128, TT], F32) for i in range(2)]
        obb = P.sb(st, "p_obb", [128, TT], BF16)
        tk = P.sb(st, "p_tk", [128, 16, 128], F32)
        tkb = P.sb(st, "p_tkb", [128, 16, 128], BF16)
        rmask = P.sb(st, "p_rmask", [16, TT], F32)
        negA = P.sb(st, "p_negA", [16, 1], F32)
        dtt = P.sb(st, "p_dtt", [16, TT], F32)
        dat = P.sb(st, "p_dat", [16, TT], F32)
        act_ = P.sb(st, "p_act", [16, TT], F32)
        dtk = P.sb(st, "p_dtk", [128, 16, 32], F32)

        for hf in range(4):
            P.dma("sp", xT.v(xT.h[:, hf * 4:(hf + 1) * 4, :]),
                  g.xT_d.v(g.xT_d.ap.rearrange("(c p) t -> p c t", p=128)[:, hf * 4:(hf + 1) * 4, :], *range(16)))
        P.dma("sp", pp[:], g.pp.v(g.pp.ap[l]))
        P.dma("sp", rmask[:], g.consts.v(g.consts.ap[0:16, C_RM:C_RM + TT]))
        for i in range(2):
            P.memset(ub[i][:, 0:4], 0.0)

        blocks = [(0, 512), (512, 512), (1024, 512), (1536, 512), (2048, 512), (2560, 16)]
        blocks += [(2576 + 512 * i, 512) for i in range(6)] + [(5648, 288)]
        nblk = len(blocks)
        wi = g.w_in.ap[l]

        def issue_load(bi):
            c0, n = blocks[bi]
            t = wt[bi % 2]
            load_wblock(P, t, wi[:, c0:c0 + n], n)
            if bi == nblk - 1 and l > 0:
                s = g.w_vres.ap[l - 1].rearrange("(c p) m -> p c m", p=128)
                P.dma("pool", t.v(t.h[:, :, n:n + 32]), V(s, []))

        chunks = []
        for j in range(8):
            chunks.append((j * 128, 128, "z", j))
        for j in range(12):
            chunks.append((1024 + j * 128, 128, "conv", j))
        chunks.append((2560, 16, "dt", 0))
        for j in range(26):
            chunks.append((2576 + j * 128, 128, "rw", j))
        chunks.append((2576 + 26 * 128, 32 + (32 if l > 0 else 0), "rw", 26))

        def blk_of(c0):
            for bi, (b0, n) in enumerate(blocks):
                if b0 <= c0 < b0 + n:
                    return bi
            raise AssertionError(c0)

        issue_load(0)
        loaded = 1
        HS = 1024
        deferred = []
        for (c0, wdt, kind, idx) in chunks:
            bi = blk_of(c0)
            off = c0 - blocks[bi][0]
            while loaded <= bi + 1 and loaded < nblk:
                issue_load(loaded)
                loaded += 1
            w_ = wt[bi % 2]
            u = ub[idx % 2]
            o = ob[idx % 2]
            for hf in range(2):
                for tq in range(2):
                    t0 = hf * HS + tq * 512
                    for kc in range(16):
                        P.mm(bigh[hf].v(bigh[hf].h[0:wdt, tq * 512:(tq + 1) * 512]), w_.v(w_.h[:, kc, off:off + wdt]),
                             xT.v(xT.h[:, kc, t0:t0 + 512]), start=(kc == 0), stop=(kc == 15))
                src = bigh[hf].v(bigh[hf].h[0:wdt, :])
                if kind == "z":
                    P.activation(o.v(o.h[:, hf * HS:(hf + 1) * HS]), src, AF.Silu)
                elif kind == "dt":
                    P.activation(dtt.v(dtt.h[:, hf * HS:(hf + 1) * HS]), src, AF.Exp, bias=pp[0:16, 60:61])
                else:
                    P.copy(u.v(u.h[0:wdt, 4 + hf * HS:4 + (hf + 1) * HS]), src, e="act")
            for fn_ in deferred:
                fn_()
            deferred = []

            def to_tok_f32_now(srcT, dst_d, col0):
                for half in range(2):
                    for tt in range(8):
                        t_ = half * 8 + tt
                        P.tr(ptr[:, tt * 128:(tt + 1) * 128], srcT[:, t_ * 128:(t_ + 1) * 128], g.ident, sig=(tt == 7))
                    P.copy(tk.v(tk.h[:, half * 8:(half + 1) * 8, :].rearrange("p c t -> p (c t)")), ptr[:, 0:1024])
                P.dma("sp", dst_d.v(dst_d.ap.rearrange("(c p) m -> p c m", p=128)[:, :, col0:col0 + 128], "all"), tk[:])

            def to_tok_f32(srcT, dst_d, col0):
                deferred.append(lambda: to_tok_f32_now(srcT, dst_d, col0))

            if kind == "z":
                to_tok_f32(o, g.zs_d, idx * 128)
            elif kind == "conv":
                cwb = idx * 4
                P.ts(acc[:], u[:, 1:1 + TT], pp[:, cwb:cwb + 1], ALU.mult, pp[:, 48 + idx:49 + idx], ALU.add)
                for k in range(1, 4):
                    P.stt(acc[:], u[:, 1 + k:1 + k + TT], pp[:, cwb + k:cwb + k + 1], acc[:], ALU.mult, ALU.add)
                if idx < 8:
                    P.activation(o[:], acc[:], AF.Silu)
                    to_tok_f32(o, g.xs_d, idx * 128)
                else:
                    P.activation(obb[:], acc[:], AF.Silu)
                    gi = (idx - 8) % 2
                    if idx < 10:
                        P.dma("sp", g.BT_d.v(g.BT_d.ap[gi * 128:(gi + 1) * 128, :], "all"), obb[:])

                        def b_post(gi=gi):
                            for tt in range(16):
                                P.tr(pbt[:, tt * 128:(tt + 1) * 128], obb[:, tt * 128:(tt + 1) * 128], g.identb[:], sig=(tt == 15))
                            P.copy(tkb.v(tkb.h[:].rearrange("p c t -> p (c t)")), pbt[:])
                            P.dma("sp", g.Btok_d.v(g.Btok_d.ap.rearrange("(c p) m -> p c m", p=128)[:, :, gi * 128:(gi + 1) * 128], "all"), tkb[:])
                        deferred.append(b_post)
                    else:
                        P.dma("sp", g.CT_d.v(g.CT_d.ap[gi * 128:(gi + 1) * 128, :], "all"), obb[:])
            elif kind == "dt":
                P.activation(dtt[:], dtt[:], AF.Ln, bias=1.0)
                P.activation(negA[:], pp[0:16, 61:62], AF.Exp)
                P.ts(negA[:], negA[:], -1.0, ALU.mult)
                P.ts(dat[:], dtt[:], negA[:, 0:1], ALU.mult)
                P.op("dve", lambda v: v.tensor_tensor_scan(act_.h[:], rmask.h[:], dat.h[:], 0.0, ALU.mult, ALU.add),
                     reads=[rmask[:], dat[:]], writes=[act_[:]])
                P.dma("sp", g.acT_d.v(g.acT_d.ap, "all"), act_[:])
                for tt in range(16):
                    P.tr(ptr[:, tt * 32:tt * 32 + 16], dtt[:, tt * 128:(tt + 1) * 128], g.cst[0:16, C_ID:C_ID + 16], sig=False)
                    P.tr(ptr[:, tt * 32 + 16:tt * 32 + 32], act_[:, tt * 128:(tt + 1) * 128], g.cst[0:16, C_ID:C_ID + 16], sig=True)
                P.copy(dtk.v(dtk.h[:].rearrange("p c t -> p (c t)")), ptr[:, 0:512])
                P.dma("sp", g.dttok_d.v(g.dttok_d.ap.rearrange("(c p) m -> p c m", p=128), "all"), dtk[:])
            elif kind == "rw":
                mcol = 62 + idx
                P.tt(acc.v(acc.h[0:wdt, :]), u.v(u.h[0:wdt, 3:3 + TT]), u.v(u.h[0:wdt, 4:4 + TT]), ALU.subtract)
                P.stt(o.v(o.h[0:wdt, :]), acc.v(acc.h[0:wdt, :]), pp[0:wdt, mcol:mcol + 1], u.v(u.h[0:wdt, 4:4 + TT]), ALU.mult, ALU.add)
                P.dma("sp", g.pT_d.v(g.pT_d.ap[idx * 128:idx * 128 + wdt, :], idx), o.v(o.h[0:wdt, :]))
        for fn_ in deferred:
            fn_()
        P.barrier()


def phase_ssd(g, l):
    P = g.P
    with contextlib.ExitStack() as st:
        BT = P.sb(st, "s_BT", [128, 2, TT], BF16)
        CT = P.sb(st, "s_CT", [128, 2, TT], BF16)
        acT = P.sb(st, "s_acT", [16, TT], F32)
        sel16 = P.sb(st, "s_sel", [16, 2048], F32)
        bvt = P.sb(st, "s_bv", [128, 1040], F32)
        H = P.sb(st, "s_H", [128, 1024], F32)
        Hb = P.sb(st, "s_Hb", [128, 1024], BF16)
        yT = P.sb(st, "s_yT", [128, 8, TT], BF16)
        xs = [P.sb(st, f"s_xs{i}", [128, 1024], F32) for i in range(2)]
        zs = [P.sb(st, f"s_zs{i}", [128, 1024], F32) for i in range(2)]
        dk = [P.sb(st, f"s_dk{i}", [128, 32], F32) for i in range(2)]
        Bk = [P.sb(st, f"s_Bk{i}", [128, 256], BF16) for i in range(2)]
        xdt = P.sb(st, "s_xdt", [128, 1024], BF16)
        xdte = P.sb(st, "s_xdte", [128, 1024], BF16)
        cbm = P.sb(st, "s_cbm", [128, 2, 128], F32)
        dif = P.sb(st, "s_dif", [128, 8, 128], F32)
        M = P.sb(st, "s_M", [128, 16, 128], BF16)
        alast = P.sb(st, "s_alast", [128, 16], F32)
        expa = P.sb(st, "s_expa", [128, 16], F32)
        dte = P.sb(st, "s_dte", [128, 16], F32)
        cd = P.sb(st, "s_cd", [128, 16], F32)
        wgt = P.sb(st, "s_wgt", [128, 16], F32)
        y1 = P.sb(st, "s_y1", [128, 1024], F32)
        y = P.sb(st, "s_y", [128, 1024], F32)
        tmp = P.sb(st, "s_tmp", [128, 1024], F32)
        ssq = P.sb(st, "s_ssq", [128, 2], F32)
        rstd = P.sb(st, "s_rstd", [128, 2], F32)
        yb = P.sb(st, "s_yb", [128, 1024], BF16)
        psA = P.ps(st, "s_psA", [128, 1024], F32)
        psB = P.ps(st, "s_psB", [128, 1024], F32)
        psC = P.ps(st, "s_psC", [128, 1024], F32)
        psD = P.ps(st, "s_psD", [128, 512], F32)
        psE = P.ps(st, "s_psE", [128, 1024], BF16)

        for gi in range(2):
            P.dma("sp", BT.v(BT.h[:, gi, :]), g.BT_d.v(g.BT_d.ap[gi * 128:(gi + 1) * 128, :], "all"))
            P.dma("sp", CT.v(CT.h[:, gi, :]), g.CT_d.v(g.CT_d.ap[gi * 128:(gi + 1) * 128, :], "all"))
        P.dma("sp", acT[:], g.acT_d.v(g.acT_d.ap, "all"))
        P.dma("sp", sel16[:], g.consts.v(g.consts.ap[0:16, C_S16:C_S16 + 2048]))
        P.dma("sp", bvt[:], g.bv.v(g.bv.ap[l, 0:1040].partition_broadcast(128)))
        P.memset(H[:], 0.0)
        P.memset(Hb[:], 0.0)
        mI = g.cst.h[:, C_MI:C_MI + 128]

        def loads(c):
            i = c % 2
            rows = slice(c * 128, (c + 1) * 128)
            P.dma("sp", xs[i][:], g.xs_d.v(g.xs_d.ap[rows, :], "all"))
            P.dma("sp", zs[i][:], g.zs_d.v(g.zs_d.ap[rows, :], "all"))
            P.dma("sp", dk[i][:], g.dttok_d.v(g.dttok_d.ap[rows, :], "all"))
            P.dma("sp", Bk[i][:], g.Btok_d.v(g.Btok_d.ap[rows, :], "all"))

        loads(0)
        for c in range(16):
            i = c % 2
            if c + 1 < 16:
                loads(c + 1)
            tsl = slice(c * 128, (c + 1) * 128)
            X, Z, DK, BK = xs[i], zs[i], dk[i], Bk[i]
            P.tt(xdt.v(r3(xdt.h[:], 64)), X.v(r3(X.h[:], 64)), DK.v(bc3(DK.h[:, 0:16], 64)), ALU.mult)
            for gi in range(2):
                P.mm(psD[:, gi * 128:(gi + 1) * 128], BT.v(BT.h[:, gi, tsl]), CT.v(CT.h[:, gi, tsl]))
            P.tt(cbm[:], psD.v(psD.h[:, 0:256].rearrange("p (a b) -> p a b", b=128)),
                 g.cst.v(mI.unsqueeze(1).to_broadcast([128, 2, 128])), ALU.mult)
            for hh in range(2):
                for e8 in range(8):
                    e = hh * 8 + e8
                    P.mm(psA[:, e8 * 128:(e8 + 1) * 128], sel16[:, e * 128:(e + 1) * 128], acT[:, tsl])
                pa3 = psA.h[:].rearrange("p (a b) -> p a b", b=128)
                P.tt(dif[:], psA.v(pa3), DK.v(bc3(DK.h[:, 16 + hh * 8:24 + hh * 8], 128)), ALU.subtract)
                P.copy(alast.v(alast.h[:, hh * 8:(hh + 1) * 8].unsqueeze(2)), psA.v(pa3[:, :, 127:128]))
                P.activation(dif[:], dif[:], AF.Exp)
                P.stt(M.v(M.h[:, hh * 8:(hh + 1) * 8, :]), dif[:], 1.0,
                      cbm.v(cbm.h[:, hh, :].unsqueeze(1).to_broadcast([128, 8, 128])), ALU.min, ALU.mult)
            for e in range(16):
                P.mm(psB[:, e * 64:(e + 1) * 64], M.v(M.h[:, e, :]), xdt[:, e * 64:(e + 1) * 64])
            for gi in range(2):
                P.mm(psC[:, gi * 512:(gi + 1) * 512], CT.v(CT.h[:, gi, tsl]), Hb[:, gi * 512:(gi + 1) * 512])
            P.activation(expa[:], DK[:, 16:32], AF.Exp)
            P.tt(y1.v(r3(y1.h[:], 64)), psC.v(r3(psC.h[:], 64)), expa.v(bc3(expa.h[:], 64)), ALU.mult)
            P.tt(y[:], y1[:], psB[:], ALU.add)
            P.tt(tmp.v(r3(tmp.h[:], 64)), X.v(r3(X.h[:], 64)), bvt.v(bc3(bvt.h[:, 0:16], 64)), ALU.mult)
            P.tt(y[:], y[:], tmp[:], ALU.add)
            P.tt(y[:], y[:], Z[:], ALU.mult)
            for gi in range(2):
                P.activation(tmp[:, gi * 512:(gi + 1) * 512], y[:, gi * 512:(gi + 1) * 512], AF.Square, accum_out=ssq[:, gi:gi + 1])
            P.activation(rstd[:], ssq[:], AF.Sqrt, bias=1e-5, scale=1.0 / 512.0)
            P.op("dve", lambda v: v.reciprocal(rstd.h[:], rstd.h[:]), reads=[rstd[:]], writes=[rstd[:]])
            for gi in range(2):
                P.ts(y1[:, gi * 512:(gi + 1) * 512], y[:, gi * 512:(gi + 1) * 512], rstd[:, gi:gi + 1], ALU.mult)
            P.tt(yb[:], y1[:], bvt[:, 16:1040], ALU.mult)
            for j in range(8):
                P.tr(psE[:, j * 128:(j + 1) * 128], yb[:, j * 128:(j + 1) * 128], g.identb[:], sig=(j == 7))
            P.copy(yT.v(yT.h[:, :, tsl]), psE.v(psE.h[:].rearrange("p (a b) -> p a b", b=128)))
            P.tt(dte[:], alast[:], DK[:, 16:32], ALU.subtract)
            P.activation(dte[:], dte[:], AF.Exp)
            P.activation(cd[:], alast[:], AF.Exp)
            P.tt(wgt[:], DK[:, 0:16], dte[:], ALU.mult)
            P.tt(xdte.v(r3(xdte.h[:], 64)), X.v(r3(X.h[:], 64)), wgt.v(bc3(wgt.h[:], 64)), ALU.mult)
            for gi in range(2):
                P.mm(psC[:, gi * 512:(gi + 1) * 512], BK[:, gi * 128:(gi + 1) * 128], xdte[:, gi * 512:(gi + 1) * 512])
            P.tt(H.v(r3(H.h[:], 64)), H.v(r3(H.h[:], 64)), cd.v(bc3(cd.h[:], 64)), ALU.mult)
            P.tt(H[:], H[:], psC[:], ALU.add)
            P.copy(Hb[:], H[:], e="act")
        P.dma("sp", g.yT_d.v(g.yT_d.ap[0:1024, :].rearrange("(c p) t -> p c t", p=128), "ssd"), yT[:])
        P.barrier()


def phase_rwkv(g, l):
    P = g.P
    U = 4
    with contextlib.ExitStack() as st:
        pp = P.sb(st, "r_pp", [128, NPP], F32)
        omk = P.sb(st, "r_omk", [128, 8], F32)
        bvt = P.sb(st, "r_bv", [128, 2048], F32)
        mk4 = P.sb(st, "r_mk4", [128, 512], F32)
        waT = P.sb(st, "r_waT", [128, TT], BF16)
        gAT = P.sb(st, "r_gAT", [128, TT], BF16)
        gBT = P.sb(st, "r_gBT", [64, TT], BF16)
        wa_up = P.sb(st, "r_waup", [128, 1024], BF16)
        gA_up = P.sb(st, "r_gAup", [128, 1024], BF16)
        gB_up = P.sb(st, "r_gBup", [64, 1024], BF16)
        arT = P.sb(st, "r_arT", [128, 16, 2, 128], BF16)
        bTb = P.sb(st, "r_bTb", [128, TT], BF16)
        kTb = P.sb(st, "r_kTb", [128, TT], BF16)
        Btok = P.sb(st, "r_Btok", [128, 16, 128], BF16)
        Ktok = P.sb(st, "r_Ktok", [128, 16, 128], BF16)
        Vtok = P.sb(st, "r_Vtok", [128, 16, 128], F32)
        Vbf = P.sb(st, "r_Vbf", [128, 16, 128], BF16)
        coef = P.sb(st, "r_coef", [128, 16, 2], F32)
        wc = P.sb(st, "r_wc", [128, 16], F32)
        yrwT = P.sb(st, "r_yrwT", [128, TT], BF16)
        rmask = P.sb(st, "r_rmask", [128, TT], F32)
        P.dma("sp", rmask[:], g.consts.v(g.consts.ap[:, C_RM:C_RM + TT]))
        Q = [P.ps(st, f"r_Q{i}", [128, 1024], F32) for i in range(3)]
        B6 = P.ps(st, "r_B6", [128, 512], F32)
        pTb = P.ps(st, "r_pTb", [128, 1024], BF16)

        def qv(qi, ap_fn, keys):
            return Q[qi].k(keys, ap_fn(Q[qi].h))

        psL = lambda sl=slice(0, 1024): qv(0, lambda h: h[:, sl], [0] if sl.stop <= 512 else ([1] if sl.start >= 512 else [0, 1]))

        P.dma("sp", pp[:], g.pp.v(g.pp.ap[l]))
        P.dma("sp", bvt[:], g.bv.v(g.bv.ap[l, BV_LW:BV_LW + 2048].partition_broadcast(128)))
        P.ts(omk[:], pp[:, 121:129], -1.0, ALU.mult, 1.0, ALU.add)
        for q, cc in enumerate((C_MS, C_MI, C_MS, C_MI)):
            P.copy(mk4[:, q * 128:(q + 1) * 128], g.cst[:, cc:cc + 128])
        P.dma("pool", wa_up.v(wa_up.h[0:64, :]), V(g.w_up.ap[l], []))
        P.dma("pool", wa_up.v(wa_up.h[64:128, :]), V(g.a_up.ap[l], []))
        P.dma("pool", gA_up[:], V(g.g_up.ap[l, 0:128, :], []))
        P.dma("pool", gB_up.v(gB_up.h[0:32, :]), V(g.g_up.ap[l, 128:160, :], []))
        if l > 0:
            P.dma("pool", gB_up.v(gB_up.h[32:64, :]), V(g.v_up.ap[l - 1], []))
        with contextlib.ExitStack() as st1:
            A = P.sb(st1, "r_A0", [128, TT], F32)
            P.dma("sp", A[:], g.pT_d.v(g.pT_d.ap[3072:3200, :], 24))
            P.activation(waT.v(waT.h[0:64, :]), A.v(A.h[0:64, :]), AF.Tanh)
            P.copy(waT.v(waT.h[64:128, :]), A.v(A.h[64:128, :]))
            P.dma("sp", A[:], g.pT_d.v(g.pT_d.ap[3200:3328, :], 25))
            P.activation(gAT[:], A[:], AF.Sigmoid)
            nr = 64 if l > 0 else 32
            P.dma("sp", A.v(A.h[0:nr, :]), g.pT_d.v(g.pT_d.ap[3328:3328 + nr, :], 26))
            P.activation(gBT.v(gBT.h[0:32, :]), A.v(A.h[0:32, :]), AF.Sigmoid)
            if l > 0:
                P.copy(gBT.v(gBT.h[32:64, :]), A.v(A.h[32:64, :]))
            P.barrier()

        HS = 1024
        for hp in range(8):
            cs = slice(hp * 128, (hp + 1) * 128)
            with contextlib.ExitStack() as st2:
                t_r = P.sb(st2, "r_tr", [128, TT], F32)
                t_k = P.sb(st2, "r_tk", [128, TT], F32)
                t_v = P.sb(st2, "r_tv", [128, TT], F32)
                A = P.sb(st2, "r_A", [128, TT], F32)
                B = P.sb(st2, "r_B", [128, TT], F32)
                C = P.sb(st2, "r_C", [128, TT], F32)
                Dd = P.sb(st2, "r_D", [128, TT], F32)
                E = P.sb(st2, "r_E", [128, TT], F32)
                F = P.sb(st2, "r_F", [128, TT], F32)
                P.dma("sp", t_r[:], g.pT_d.v(g.pT_d.ap[hp * 128:(hp + 1) * 128, :], hp))
                P.dma("sp", t_k[:], g.pT_d.v(g.pT_d.ap[1024 + hp * 128:1024 + (hp + 1) * 128, :], 8 + hp))
                P.dma("sp", t_v[:], g.pT_d.v(g.pT_d.ap[2048 + hp * 128:2048 + (hp + 1) * 128, :], 16 + hp))

                def lora(dst, up, rows, src, bias_col):
                    for hf in range(2):
                        for tq in range(2):
                            t0 = hf * HS + tq * 512
                            P.mm(psL(slice(tq * 512, (tq + 1) * 512)), up.v(up.h[rows, cs]), src.v(src.h[rows, t0:t0 + 512]))
                        P.activation(dst[:, hf * HS:(hf + 1) * HS], psL(), AF.Sigmoid, bias=pp[:, bias_col:bias_col + 1])

                lora(A, wa_up, slice(0, 64), waT, 89 + hp)
                P.ts(A[:], A[:], -0.6065306597126334, ALU.mult)
                P.op("dve", lambda v: v.tensor_tensor_scan(B.h[:], rmask.h[:], A.h[:], 0.0, ALU.mult, ALU.add),
                     reads=[rmask[:], A[:]], writes=[B[:]])
                P.tt(A[:], B[:], A[:], ALU.subtract)
                P.activation(A[:], A[:], AF.Exp)
                P.activation(C[:], B[:], AF.Exp)
                P.activation(Dd[:], B[:], AF.Exp, scale=-1.0)
                P.copy(wc.v(wc.h[:].unsqueeze(2)), C.v(C.h[:].rearrange("p (c l) -> p c l", l=128)[:, :, 127:128]))
                lora(B, wa_up, slice(64, 128), waT, 97 + hp)
                if l > 0:
                    lora(E, gB_up, slice(32, 64), gBT, 105 + hp)
                    P.dma("sp", F[:], g.vfT_d.v(g.vfT_d.ap[cs, :], hp))
                    P.tt(F[:], F[:], t_v[:], ALU.subtract)
                    P.tt(F[:], F[:], E[:], ALU.mult)
                    P.tt(t_v[:], t_v[:], F[:], ALU.add)
                else:
                    P.dma("sp", g.vfT_d.v(g.vfT_d.ap[cs, :], hp), t_v[:])
                P.ts(E[:], t_k[:], pp[:, 113 + hp:114 + hp], ALU.mult)
                P.tt(F[:], E[:], E[:], ALU.mult)
                for hf in range(2):
                    for tq in range(2):
                        t0 = hf * HS + tq * 512
                        P.mm(psL(slice(tq * 512, (tq + 1) * 512)), g.cst[:, C_BO:C_BO + 128], F[:, t0:t0 + 512])
                    P.ts(F[:, hf * HS:(hf + 1) * HS], psL(), 1e-24, ALU.max)
                P.activation(F[:], F[:], AF.Ln)
                P.activation(F[:], F[:], AF.Exp, scale=-0.5)
                P.tt(E[:], E[:], F[:], ALU.mult)
                P.ts(F[:], B[:], pp[:, 121 + hp:122 + hp], ALU.mult, omk[:, hp:hp + 1], ALU.add)
                P.tt(F[:], t_k[:], F[:], ALU.mult)
                P.tt(t_k[:], t_r[:], F[:], ALU.mult)
                P.ts(t_k[:], t_k[:], pp[:, 129 + hp:130 + hp], ALU.mult)
                for tt_ in range(16):
                    P.mm(psL(slice(tt_ * 2, tt_ * 2 + 2)), t_k[:, tt_ * 128:(tt_ + 1) * 128], g.cst[:, C_S2:C_S2 + 2])
                P.copy(coef.v(coef.h[:].rearrange("p c h -> p (c h)")), psL(slice(0, 32)))
                a3 = lambda t: t.h[:].rearrange("p (c l) -> p c l", l=128)
                P.stt(arT.v(arT.h[:, :, 0, :]), E.v(a3(E)), -1.0, A.v(a3(A)), ALU.mult, ALU.mult)
                P.tt(arT.v(arT.h[:, :, 1, :]), t_r.v(a3(t_r)), C.v(a3(C)), ALU.mult)
                P.tt(B[:], E[:], B[:], ALU.mult)
                P.tt(bTb[:], B[:], Dd[:], ALU.mult)
                P.tt(kTb[:], F[:], Dd[:], ALU.mult)
                for (src, dst) in ((bTb, Btok), (kTb, Ktok)):
                    for half in range(2):
                        for j in range(8):
                            t_ = half * 8 + j
                            P.tr(pTb[:, j * 128:(j + 1) * 128], src[:, t_ * 128:(t_ + 1) * 128], g.identb[:], sig=(j == 7))
                        P.copy(dst.v(dst.h[:, half * 8:(half + 1) * 8, :].rearrange("p c t -> p (c t)")), pTb[:], e="act")
                for half in range(2):
                    for j in range(8):
                        t_ = half * 8 + j
                        P.tr(psL(slice(j * 128, (j + 1) * 128)), t_v[:, t_ * 128:(t_ + 1) * 128], g.ident, sig=(j == 7))
                    P.copy(Vtok.v(Vtok.h[:, half * 8:(half + 1) * 8, :].rearrange("p c t -> p (c t)")), psL())
                P.copy(Vbf[:], Vtok[:], e="act")
                P.barrier()

            with contextlib.ExitStack() as st3:
                A4a = P.sb(st3, "r_A4a", [128, 16, 2, 512], BF16)
                XTa = P.sb(st3, "r_XTa", [128, 16, 2, 128], BF16)
                Pl = [[P.sb(st3, f"r_Pl{u}{i}", [128, 2, 2, 128], BF16) for i in range(2)] for u in range(U)]
                XTf = [P.sb(st3, f"r_XTf{u}", [128, 2, 128], F32) for u in range(U)]
                Hf = P.sb(st3, "r_Hf", [128, 64], F32)
                tmpH = P.sb(st3, "r_tmpH", [128, 64], F32)
                Hblk = P.sb(st3, "r_Hblk", [128, 128], BF16)
                RHSb = P.sb(st3, "r_RHSb", [128, 128], BF16)
                Ub = [P.sb(st3, f"r_Ub{i}", [128, 128], BF16) for i in range(2)]
                ysb = [P.sb(st3, f"r_ysb{i}", [128, 128], F32) for i in range(2)]
                ysq = P.sb(st3, "r_ysq", [128, 128], F32)
                yn = P.sb(st3, "r_yn", [128, 128], F32)
                yo = P.sb(st3, "r_yo", [128, 128], BF16)
                stt_ = P.sb(st3, "r_st", [128, 12], F32)
                A4 = lambda c, sl=slice(0, 512), h=None: (A4a.k(("c", c), A4a.h[:, c, :, sl]) if h is None
                                                           else A4a.k(("c", c), A4a.h[:, c, h, sl]))
                XT = lambda c, h=None: (XTa.k(("c", c), XTa.h[:, c, :, :]) if h is None else XTa.k(("c", c), XTa.h[:, c, h, :]))
                mL = g.cst[:, C_ML:C_ML + 128]
                idb = g.cst.v(g.cst.h[:, C_ID:C_ID + 128].unsqueeze(1).to_broadcast([128, 2, 128]))

                def pLv(u):
                    qi, kk_ = u // 2, u % 2
                    return lambda fn: qv(qi, lambda h: fn(h[:, kk_ * 512:(kk_ + 1) * 512].rearrange("p (h a b) -> p h a b", h=2, a=2)), [kk_])

                def pX(u):
                    kk_, off = u // 2, (u % 2) * 256
                    return lambda fn: qv(2, lambda h: fn(h[:, kk_ * 512 + off:kk_ * 512 + off + 256].rearrange("p (h b) -> p h b", h=2)), [kk_])

                for g0 in range(0, 16, U):
                    units = list(range(g0, g0 + U))
                    for u, c in enumerate(units):
                        tsl = slice(c * 128, (c + 1) * 128)
                        for h in range(2):
                            pr = slice(h * 64, (h + 1) * 64)
                            ar = arT.v(arT.h[pr, c, :, :].rearrange("p a b -> p (a b)"))
                            P.mm(qv(0, lambda hh: hh[:, h * 512:h * 512 + 256], [h]), bTb[pr, tsl], ar)
                            P.mm(qv(0, lambda hh: hh[:, h * 512 + 256:h * 512 + 512], [h]), kTb[pr, tsl], ar)
                            P.mm(qv(1, lambda hh: hh[:, h * 512:h * 512 + 128], [h]), arT.v(arT.h[pr, c, 0, :]), bTb[pr, tsl])
                        for h in range(2):
                            P.tt(A4(c, h=h), qv(0, lambda hh: hh[:, h * 512:(h + 1) * 512], [h]), mk4[:], ALU.mult)
                            P.tt(Pl[u][0].v(Pl[u][0].h[:, h, 0, :]), qv(1, lambda hh: hh[:, h * 512:h * 512 + 128], [h]), mL, ALU.mult)
                        P.copy(Pl[u][0].v(Pl[u][0].h[:, :, 1, :]), A4(c, slice(0, 128)), e="act")
                        P.tt(XTf[u][:], A4(c, slice(0, 128)), idb, ALU.add)
                        P.copy(XT(c), XTf[u][:], e="act")
                    for k in range(1, 7):
                        for u, c in enumerate(units):
                            cur = Pl[u][(k - 1) % 2]
                            for h in range(2):
                                P.mm(pLv(u)(lambda a: a[:, h, 0, :]), cur.v(cur.h[:, h, 1, :]), cur.v(cur.h[:, h, 0, :]))
                                if k < 6:
                                    P.mm(pLv(u)(lambda a: a[:, h, 1, :]), cur.v(cur.h[:, h, 0, :]), cur.v(cur.h[:, h, 1, :]))
                        for u, c in enumerate(units):
                            nxt = Pl[u][k % 2]
                            if k < 6:
                                P.copy(nxt[:], pLv(u)(lambda a: a), e="act")
                            else:
                                P.copy(nxt.v(nxt.h[:, :, 0, :]), pLv(u)(lambda a: a[:, :, 0, :]), e="act")
                        for u, c in enumerate(units):
                            nxt = Pl[u][k % 2]
                            for h in range(2):
                                P.mm(pX(u)(lambda a: a[:, h, :]), nxt.v(nxt.h[:, h, 0, :]), XT(c, h))
                        for u, c in enumerate(units):
                            P.tt(XTf[u][:], XTf[u][:], pX(u)(lambda a: a), ALU.add)
                        for u, c in enumerate(units):
                            P.copy(XT(c), XTf[u][:], e="act")

                P.memset(Hf[:], 0.0)
                P.memset(Hblk[:], 0.0)
                pS = lambda sl: B6.v(B6.h[:, sl])

                def chain(c):
                    UB = Ub[c % 2]
                    P.mm(pS(slice(0, 128)), arT.v(arT.h[:, c, 0, :]), Hblk[:], start=True, stop=False)
                    for h in range(2):
                        hs = slice(h * 64, (h + 1) * 64)
                        P.mm(pS(slice(h * 64, (h + 1) * 64)), A4(c, slice(256, 384), h), Vbf.v(Vbf.h[:, c, hs]), start=False, stop=(h == 1))
                    P.copy(RHSb[:], pS(slice(0, 128)))
                    for h in range(2):
                        hs = slice(h * 64, (h + 1) * 64)
                        P.mm(pS(slice(128 + h * 64, 128 + (h + 1) * 64)), XT(c, h), RHSb[:, hs])
                    P.copy(UB[:], pS(slice(128, 256)), e="act")
                    P.mm(pS(slice(256, 384)), arT.v(arT.h[:, c, 1, :]), Hblk[:], start=True, stop=False)
                    for h in range(2):
                        hs = slice(h * 64, (h + 1) * 64)
                        P.mm(pS(slice(256 + h * 64, 256 + (h + 1) * 64)), A4(c, slice(128, 256), h), UB[:, hs], start=False, stop=False)
                        P.mm(pS(slice(256 + h * 64, 256 + (h + 1) * 64)), A4(c, slice(384, 512), h), Vbf.v(Vbf.h[:, c, hs]), start=False, stop=(h == 1))
                    P.mm(pS(slice(384, 512)), Btok.v(Btok.h[:, c, :]), UB[:], start=True, stop=False)
                    P.mm(pS(slice(384, 512)), Ktok.v(Ktok.h[:, c, :]), Vbf.v(Vbf.h[:, c, :]), start=False, stop=True)
                    for h in range(2):
                        pr = slice(h * 64, (h + 1) * 64)
                        P.ts(tmpH[pr, :], Hf[pr, :], wc[pr, c:c + 1], ALU.mult)
                        P.stt(Hf[pr, :], B6.v(B6.h[pr, 384 + h * 64:384 + (h + 1) * 64]),
                              wc[pr, c:c + 1], tmpH[pr, :], ALU.mult, ALU.add)
                        P.copy(Hblk[pr, h * 64:(h + 1) * 64], Hf[pr, :], e="act")
                    P.copy(ysb[c % 2][:], pS(slice(256, 384)), e="act")

                def epi(c):
                    tsl = slice(c * 128, (c + 1) * 128)
                    Y = ysb[c % 2]
                    y3 = Y.h[:].rearrange("p (h i) -> p h i", i=64)
                    P.op("dve", lambda v: v.tensor_reduce(stt_.h[:, 0:2], y3, AX.X, ALU.add), reads=[Y[:]], writes=[stt_[:]])
                    P.tt(ysq[:], Y[:], Y[:], ALU.mult)
                    q3 = ysq.h[:].rearrange("p (h i) -> p h i", i=64)
                    P.op("dve", lambda v: v.tensor_reduce(stt_.h[:, 2:4], q3, AX.X, ALU.add), reads=[ysq[:], stt_[:]], writes=[stt_[:]])
                    P.ts(stt_[:, 4:8], stt_[:, 0:4], 1.0 / 64.0, ALU.mult)
                    P.tt(stt_[:, 8:10], stt_[:, 4:6], stt_[:, 4:6], ALU.mult)
                    P.tt(stt_[:, 8:10], stt_[:, 6:8], stt_[:, 8:10], ALU.subtract)
                    P.activation(stt_[:, 8:10], stt_[:, 8:10], AF.Sqrt, bias=64e-5)
                    P.op("dve", lambda v: v.reciprocal(stt_.h[:, 8:10], stt_.h[:, 8:10]), reads=[stt_[:]], writes=[stt_[:]])
                    P.tt(stt_[:, 10:12], stt_[:, 4:6], stt_[:, 8:10], ALU.mult)
                    for h in range(2):
                        hs = slice(h * 64, (h + 1) * 64)
                        P.ts(yn[:, hs], Y[:, hs], stt_[:, 8 + h:9 + h], ALU.mult, stt_[:, 10 + h:11 + h], ALU.subtract)
                    P.tt(yn[:], yn[:], bvt[:, hp * 128:(hp + 1) * 128], ALU.mult)
                    P.tt(yn[:], yn[:], bvt[:, 1024 + hp * 128:1024 + (hp + 1) * 128], ALU.add)
                    for h in range(2):
                        hs = slice(h * 64, (h + 1) * 64)
                        P.stt(yn[:, hs], Vtok.v(Vtok.h[:, c, hs]), coef.v(coef.h[:, c, h:h + 1]), yn[:, hs], ALU.mult, ALU.add)
                    gps = qv(2, lambda hh: hh[:, 0:128], [0])
                    P.mm(gps, gAT[:, tsl], gA_up[:, cs], start=True, stop=False)
                    P.mm(gps, gBT.v(gBT.h[0:32, tsl]), gB_up.v(gB_up.h[0:32, cs]), start=False, stop=True)
                    P.tt(yo[:], yn[:], gps, ALU.mult)
                    P.tr(pTb[:, 0:128], yo[:], g.identb[:])
                    P.copy(yrwT[:, tsl], pTb[:, 0:128], e="act")

                chain(0)
                for c in range(1, 16):
                    chain(c)
                    epi(c - 1)
                epi(15)
                P.dma("sp", g.yT_d.v(g.yT_d.ap[1024 + hp * 128:1024 + (hp + 1) * 128, :], "rw%d" % hp), yrwT[:])
                P.barrier()


def ln_tiles(P, st, pfx, nt=1):
    d = Ctx()
    d.ts_ = [P.sb(st, pfx + f"_t{i}", [128, D], F32) for i in range(nt)]
    d.t = d.ts_[0]
    d.xb = P.sb(st, pfx + "_xb", [128, D], BF16)
    d.xo = P.sb(st, pfx + "_xo", [128, 16, 128], BF16)
    d.bst = P.sb(st, pfx + "_bst", [128, 4, 6], F32)
    d.mv = P.sb(st, pfx + "_mv", [128, 2], F32)
    d.pT = P.ps(st, pfx + "_pT", [128, 1024], BF16)
    return d


def ln_epilogue_gen(g, P, d, t, tt, gb, dst, write_xT):
    for q in range(4):
        P.op("dve", lambda v, q=q: v.bn_stats(d.bst.h[:, q, :], t.h[:, q * 512:(q + 1) * 512]), reads=[t[:]], writes=[d.bst[:]])
    P.op("dve", lambda v: v.bn_aggr(d.mv.h[:], d.bst.h[:].rearrange("p a b -> p (a b)")), reads=[d.bst[:]], writes=[d.mv[:]])
    P.activation(d.mv[:, 1:2], d.mv[:, 1:2], AF.Sqrt, bias=1e-5)
    P.op("dve", lambda v: v.reciprocal(d.mv.h[:, 1:2], d.mv.h[:, 1:2]), reads=[d.mv[:]], writes=[d.mv[:]])
    yield
    P.ts(t[:], t[:], d.mv[:, 0:1], ALU.subtract, d.mv[:, 1:2], ALU.mult)
    yield
    P.tt(t[:], t[:], gb[0], ALU.mult)
    yield
    P.tt(t[:], t[:], gb[1], ALU.add)
    rows = slice(tt * 128, (tt + 1) * 128)
    P.dma("sp", dst.v(dst.ap[rows, :], tt), t[:])
    if write_xT:
        P.copy(d.xb[:], t[:], e="act")
        yield
        for half in range(2):
            for j in range(8):
                c = half * 8 + j
                P.tr(d.pT[:, j * 128:(j + 1) * 128], d.xb[:, c * 128:(c + 1) * 128], g.identb[:], sig=(j == 7))
            P.copy(d.xo.v(d.xo.h[:, half * 8:(half + 1) * 8, :].rearrange("p c t -> p (c t)")), d.pT[:], e="act")
        P.dma("sp", g.xT_d.v(g.xT_d.ap.rearrange("(c p) t -> p c t", p=128)[:, :, rows], tt), d.xo[:])
    yield


def run_gen(gen):
    for _ in gen:
        pass


def phase_wout_ln(g, l):
    P = g.P
    xsrc = g.x if l == 0 else g.xres
    with contextlib.ExitStack() as st:
        wo = P.sb(st, "o_wo", [128, 16, D], BF16)
        bvt = P.sb(st, "o_bv", [128, 4096], F32)
        yq = [P.sb(st, f"o_yq{i}", [128, 16, 512], BF16) for i in range(2)]
        xr = [P.sb(st, f"o_xr{i}", [128, D], F32) for i in range(2)]
        ps = P.ps(st, "o_ps", [128, D], F32)
        d = ln_tiles(P, st, "o", nt=2)
        src = g.w_out.ap[l].rearrange("(c p) m -> p c m", p=128)
        for kh in range(4):
            for ch in range(2):
                P.dma("pool", wo.v(wo.h[:, kh * 4:(kh + 1) * 4, ch * 1024:(ch + 1) * 1024]),
                      V(src[:, kh * 4:(kh + 1) * 4, ch * 1024:(ch + 1) * 1024], []))
        P.dma("sp", bvt[:], g.bv.v(g.bv.ap[l, BV_G1:BV_G1 + 4096].partition_broadcast(128)))
        yv = g.yT_d.ap.rearrange("(c p) t -> p c t", p=128)

        def ldq(q):
            P.dma("sp", yq[q % 2][:], g.yT_d.v(yv[:, :, q * 512:(q + 1) * 512], "ssd", *["rw%d" % i for i in range(8)]))

        def mm_tile(tt):
            q, j = tt // 4, tt % 4
            if j == 0 and q + 1 < 4:
                ldq(q + 1)
            rows = slice(tt * 128, (tt + 1) * 128)
            P.dma("sp", xr[tt % 2][:], xsrc.v(xsrc.ap[rows, :], tt))
            Y = yq[q % 2]
            for db in range(4):
                for kc in range(16):
                    P.mm(ps[:, db * 512:(db + 1) * 512], Y.v(Y.h[:, kc, j * 128:(j + 1) * 128]), wo.v(wo.h[:, kc, db * 512:(db + 1) * 512]),
                         start=(kc == 0), stop=(kc == 15))
        ldq(0)
        mm_tile(0)
        for tt in range(16):
            t = d.ts_[tt % 2]
            P.stt(t[:], xr[tt % 2][:], ALPHA, ps[:], ALU.mult, ALU.add)
            if tt + 1 < 16:
                mm_tile(tt + 1)
            run_gen(ln_epilogue_gen(g, P, d, t, tt, (bvt[:, 0:2048], bvt[:, 2048:4096]), g.xres, True))
        P.barrier()


def phase_ffn(g, l, last):
    P = g.P
    moe = (l % 2 == 1)
    li = l // 2
    if moe:
        groups = [(g.moe_w1.ap[li, e], g.moe_w3.ap[li, e], g.moe_w2.ap[li, e]) for e in range(8)]
    else:
        groups = [(g.ffn_w1.ap[li][:, gi * DFE:(gi + 1) * DFE], g.ffn_w3.ap[li][:, gi * DFE:(gi + 1) * DFE],
                   g.ffn_w2.ap[li][gi * DFE:(gi + 1) * DFE, :]) for gi in range(2)]
    dst = g.out if last else g.xres
    NFB = 22
    with contextlib.ExitStack() as st:
        bvt = P.sb(st, "f_bv", [128, 4096], F32)
        xTq = P.sb(st, "f_xTq", [128, 16, 512], BF16)
        x1 = [P.sb(st, f"f_x1{i}", [128, D], F32) for i in range(2)]
        acc = P.sb(st, "f_acc", [128, 4, D], F32)
        w13 = [[P.sb(st, f"f_w{a}{i}", [128, 16, 128], BF16) for i in range(2)] for a in (1, 3)]
        hT = P.sb(st, "f_hT", [128, NFB, 512], BF16)
        w2t = [P.sb(st, f"f_w2{i}", [128, 11, 256], BF16) for i in range(4)]
        gact = [P.sb(st, f"f_ga{i}", [128, 512], F32) for i in range(2)]
        ps13 = [[P.ps(st, f"f_ps{a}{i}", [128, 512], F32) for i in range(2)] for a in (1, 3)]
        psw2 = [P.ps(st, f"f_psw{i}", [128, 2, 256], F32) for i in range(2)]
        d = ln_tiles(P, st, "f")
        x1e = P.sb(st, "f_x1e", [128, D], F32)
        if moe:
            junk = P.sb(st, "f_junk", [128, D], F32)
            rt = P.sb(st, "f_rt", [128, D], F32)
            lg = P.sb(st, "f_lg", [128, 4, 8], F32)
            comb = P.sb(st, "f_comb", [128, 4, 8], F32)
            m8 = P.sb(st, "f_m8", [128, 8], F32)
            gg = P.sb(st, "f_gg", [128, 2], F32)
            eq = P.sb(st, "f_eq", [128, 8], F32)
        P.dma("sp", bvt[:], g.bv.v(g.bv.ap[l, BV_G2:BV_G2 + 4096].partition_broadcast(128)))
        xv = g.xT_d.ap.rearrange("(c p) t -> p c t", p=128)
        def router_gen(q):
            for e in range(8):
                P.dma("sp", rt[:], g.routerT.v(g.routerT.ap[li, e].partition_broadcast(128)))
                for j in range(4):
                    tt = q * 4 + j
                    P.dma("sp", x1[j % 2][:], g.xres.v(g.xres.ap[tt * 128:(tt + 1) * 128, :], tt))
                    P.stt(junk[:], x1[j % 2][:], 1.0, rt[:], ALU.mult, ALU.mult, accum_out=lg.v(lg.h[:, j, e:e + 1]))
                    yield
            for j in range(4):
                P.op("dve", lambda v, j=j: v.max(m8.h[:], lg.h[:, j, :]), reads=[lg[:]], writes=[m8[:]])
                P.tt(gg[:, 0:1], m8[:, 0:1], m8[:, 1:2], ALU.subtract)
                P.activation(gg[:, 0:1], gg[:, 0:1], AF.Sigmoid)
                P.ts(gg[:, 1:2], gg[:, 0:1], -1.0, ALU.mult, 1.0, ALU.add)
                P.ts(eq[:], lg.v(lg.h[:, j, :]), m8[:, 0:1], ALU.is_equal)
                P.ts(comb.v(comb.h[:, j, :]), eq[:], gg[:, 0:1], ALU.mult)
                P.ts(eq[:], lg.v(lg.h[:, j, :]), m8[:, 1:2], ALU.is_equal)
                P.stt(comb.v(comb.h[:, j, :]), eq[:], gg[:, 1:2], comb.v(comb.h[:, j, :]), ALU.mult, ALU.add)
                yield

        def epi_gen(q):
            for j in range(4):
                tt = q * 4 + j
                t = d.ts_[0]
                P.dma("sp", x1e[:], g.xres.v(g.xres.ap[tt * 128:(tt + 1) * 128, :], tt))
                P.stt(t[:], x1e[:], ALPHA, acc.v(acc.h[:, j, :]), ALU.mult, ALU.add)
                yield
                yield from ln_epilogue_gen(g, P, d, t, tt, (bvt[:, 0:2048], bvt[:, 2048:4096]), dst, not last)

        import itertools
        pending = iter(())
        for q in range(4):
            P.dma("sp", xTq[:], g.xT_d.v(xv[:, :, q * 512:(q + 1) * 512], *range(q * 4, q * 4 + 4)))
            if moe:
                pending = itertools.chain(pending, router_gen(q))
            for gi, (w1a, w3a, w2a) in enumerate(groups):
                w1v = w1a.rearrange("(c p) m -> p c m", p=128)
                w3v = w3a.rearrange("(c p) m -> p c m", p=128)
                w2v = w2a.rearrange("(f p) m -> p f m", p=128)

                def c13v(a, fb):
                    r0 = ((a * 8 + gi) * 22 + fb) * 128
                    return g.c13.v(g.c13.ap[r0:r0 + 128, :].rearrange("p (c m) -> p c m", m=128), ("c13", a, gi, fb))

                def ldw(fb):
                    if q == 0:
                        P.dma("pool", w13[0][fb % 2][:], V(w1v[:, :, fb * 128:(fb + 1) * 128], []))
                        P.dma("pool", w13[1][fb % 2][:], V(w3v[:, :, fb * 128:(fb + 1) * 128], []))
                    else:
                        P.dma("sp", w13[0][fb % 2][:], c13v(0, fb))
                        P.dma("sp", w13[1][fb % 2][:], c13v(1, fb))
                ldw(0)
                for fb in range(NFB):
                    if fb + 1 < NFB:
                        ldw(fb + 1)
                    i = fb % 2
                    for a in range(2):
                        for kc in range(16):
                            P.mm(ps13[a][i][:], w13[a][i].v(w13[a][i].h[:, kc, :]), xTq.v(xTq.h[:, kc, :]),
                                 start=(kc == 0), stop=(kc == 15))
                    if q == 0:
                        for a in range(2):
                            P.dma("sp", c13v(a, fb), w13[a][i][:])
                    P.activation(gact[i][:], ps13[0][i][:], AF.Silu)
                    P.tt(hT.v(hT.h[:, fb, :]), gact[i][:], ps13[1][i][:], ALU.mult)
                    if gi == 0:
                        for _ in range(3):
                            next(pending, None)
                if gi == 0:
                    run_gen(pending)
                    pending = iter(())
                nld = 0
                pend_st = []
                for dp in range(8):
                    cols = slice(dp * 256, (dp + 1) * 256)
                    wts = []
                    for hf in range(2):
                        wtile = w2t[nld % 4]
                        nld += 1
                        r0 = ((gi * 8 + dp) * 2 + hf) * 128
                        cv = g.c2.v(g.c2.ap[r0:r0 + 128, :].rearrange("p (f m) -> p f m", m=256), ("c2", gi, dp, hf))
                        if q == 0:
                            P.dma("pool", wtile[:], V(w2v[:, hf * 11:(hf + 1) * 11, cols], []))
                            pend_st.append((cv, wtile))
                        else:
                            P.dma("sp", wtile[:], cv)
                        wts.append(wtile)
                    for jh in range(2):
                        psw = psw2[jh]
                        for j2 in range(2):
                            j = jh * 2 + j2
                            for fb in range(NFB):
                                wtile = wts[fb // 11]
                                P.mm(psw.v(psw.h[:, j2, :]), hT.v(hT.h[:, fb, j * 128:(j + 1) * 128]), wtile.v(wtile.h[:, fb % 11, :]),
                                     start=(fb == 0), stop=(fb == NFB - 1))
                        if jh == 1:
                            for (cv_, wt_) in pend_st:
                                P.dma("sp", cv_, wt_[:])
                            pend_st.clear()
                        a3 = acc.v(acc.h[:, jh * 2:jh * 2 + 2, cols])
                        if moe:
                            for j2 in range(2):
                                j = jh * 2 + j2
                                aj = acc.v(acc.h[:, j, cols])
                                if gi == 0:
                                    P.ts(aj, psw.v(psw.h[:, j2, :]), comb.v(comb.h[:, j, gi:gi + 1]), ALU.mult)
                                else:
                                    P.stt(aj, psw.v(psw.h[:, j2, :]), comb.v(comb.h[:, j, gi:gi + 1]), aj, ALU.mult, ALU.add)
                        else:
                            if gi == 0:
                                P.copy(a3, psw[:])
                            else:
                                P.tt(a3, a3, psw[:], ALU.add)
            pending = epi_gen(q)
        run_gen(pending)
        P.barrier()


def make_consts():
    c = np.zeros((128, CW), np.float32)
    r = np.arange(128)[:, None]
    q = np.arange(128)[None, :]
    c[:, C_ID:C_ID + 128] = (r == q)
    c[:, C_MI:C_MI + 128] = (q >= r)
    c[:, C_MS:C_MS + 128] = (q > r)
    c[:, C_ML:C_ML + 128] = (q < r)
    c[:, C_BO:C_BO + 128] = ((r // 64) == (q // 64))
    c[:, C_S2:C_S2 + 2] = ((r // 64) == np.arange(2)[None, :])
    s16 = np.zeros((128, 16, 128), np.float32)
    for e in range(16):
        s16[e, e, :] = 1.0
    c[:, C_S16:C_S16 + 2048] = s16.reshape(128, 2048)
    rm = np.ones((128, TT), np.float32)
    rm[:, ::128] = 0.0
    c[:, C_RM:C_RM + TT] = rm
    return c


def pack_params(inp):
    pp = np.zeros((NL, 128, NPP), np.float32)
    bv = np.zeros((NL, NBV), np.float32)

    def cols(vec):
        return np.ascontiguousarray(vec.reshape(-1, 128).T)

    for l in range(NL):
        cw = inp["ssd_conv_w"][l]
        pp[l, :, 0:48] = cw.reshape(4, 12, 128).transpose(2, 1, 0).reshape(128, 48)
        pp[l, :, 48:60] = cols(inp["ssd_conv_b"][l])
        pp[l, 0:16, 60] = inp["ssd_dt_bias"][l]
        pp[l, 0:16, 61] = inp["ssd_a_log"][l]
        mix = inp["rw_mix"][l]
        pp[l, :, 62:86] = cols(mix[0:3072])
        pp[l, :, 86] = mix[3072:3200]
        pp[l, :, 87] = mix[3200:3328]
        pp[l, 0:32, 88] = mix[3328:3360]
        if l > 0:
            pp[l, 32:64, 88] = inp["rw_vres_mix"][l - 1]
            pp[l, :, 105:113] = cols(inp["rw_v0"][l - 1])
        pp[l, :, 89:97] = cols(inp["rw_w0"][l])
        pp[l, :, 97:105] = cols(inp["rw_a0"][l])
        pp[l, :, 113:121] = cols(inp["rw_k_k"][l])
        pp[l, :, 121:129] = cols(inp["rw_k_a"][l])
        pp[l, :, 129:137] = cols(inp["rw_r_k"][l].reshape(-1))
        bv[l, BV_D:BV_D + 16] = inp["ssd_d"][l]
        bv[l, BV_NW:BV_NW + 1024] = inp["ssd_norm_w"][l]
        bv[l, BV_LW:BV_LW + 1024] = inp["rw_ln_w"][l]
        bv[l, BV_LB:BV_LB + 1024] = inp["rw_ln_b"][l]
        bv[l, BV_G1:BV_G1 + 2048] = inp["ln1_g"][l]
        bv[l, BV_B1:BV_B1 + 2048] = inp["ln1_b"][l]
        bv[l, BV_G2:BV_G2 + 2048] = inp["ln2_g"][l]
        bv[l, BV_B2:BV_B2 + 2048] = inp["ln2_b"][l]
    return pp, bv


def make_in_maps(inp, cores):
    f = lambda a: np.ascontiguousarray(np.asarray(a, dtype=np.float32))
    pp, bv = pack_params({k: np.asarray(v) for k, v in inp.items()})
    shared = {
        "w_in": f(inp["w_in"]), "w_in_vres": f(inp["w_in_vres"]), "rw_w_up": f(inp["rw_w_up"]),
        "rw_a_up": f(inp["rw_a_up"]), "rw_v_up": f(inp["rw_v_up"]), "rw_g_up": f(inp["rw_g_up"]),
        "w_out": f(inp["w_out"]), "ffn_w1": f(inp["ffn_w1"]), "ffn_w3": f(inp["ffn_w3"]), "ffn_w2": f(inp["ffn_w2"]),
        "moe_w1": f(inp["moe_w1"]), "moe_w3": f(inp["moe_w3"]), "moe_w2": f(inp["moe_w2"]),
        "routerT": f(np.transpose(np.asarray(inp["moe_router"]), (0, 2, 1))),
        "pp": pp, "bv": bv, "consts": make_consts(),
    }
    x = np.asarray(inp["x"], dtype=np.float32)
    return [dict(shared, x=np.ascontiguousarray(x[c])) for c in cores]


def kernel(**inputs):
    nc = build_program()
    in_maps = make_in_maps(inputs, list(range(8)))
    res = run_bass_kernel_spmd(nc, in_maps, core_ids=list(range(8)))
    return np.stack([np.asarray(r["out"], dtype=np.float32) for r in res.results], axis=0)
```
